# Optimizing a Trainium2 kernel written in Bass

```python
import math
import jax, jax.numpy as jnp
from jax import lax
import numpy as np

D_MODEL = 1024
BATCH = 8
SEQ = 2048
DEPTH = 1

D_MIX = D_MODEL
D_MLSTM = D_MIX // 2
N_HEADS_MLSTM = 4
HEAD_DIM_MLSTM = D_MLSTM // N_HEADS_MLSTM
D_MOBA = D_MIX - D_MLSTM
N_HEADS_MOBA = 8
HEAD_DIM_MOBA = D_MOBA // N_HEADS_MOBA
CONV_K = 4
MLSTM_CHUNK = 64
MOBA_BLOCK = 256
MOBA_TOPK = 3
MOBA_Q_CHUNK = 16
EPS = 1e-6
SPLIT_SIZES = [D_MLSTM, D_MLSTM, D_MLSTM, D_MLSTM, N_HEADS_MLSTM, N_HEADS_MLSTM, D_MLSTM,
               D_MOBA, D_MOBA, D_MOBA, D_MOBA]
PROJ_DIM = sum(SPLIT_SIZES)

kernel_name = "hymba_mlstm_moba_adaln_layer"


def rms_norm(x, g):
    x32 = x.astype(jnp.float32)
    return x32 * lax.rsqrt(jnp.mean(x32 * x32, axis=-1, keepdims=True) + EPS) * g.astype(jnp.float32)


def to_heads(u, n_heads):
    b, s, _ = u.shape
    return u.reshape(b, s, n_heads, -1).transpose(0, 2, 1, 3)


def causal_short_conv(u, w, bias):
    s = u.shape[1]
    u_pad = jnp.pad(u, ((0, 0), (CONV_K - 1, 0), (0, 0)))
    out = bias.astype(jnp.float32)
    for tap in range(CONV_K):
        out = out + u_pad[:, tap:tap + s] * w[tap].astype(jnp.float32)
    return out


def alibi_slopes(n_heads):
    return jnp.asarray([2.0 ** (-8.0 * (h + 1) / n_heads) for h in range(n_heads)], dtype=jnp.float32)


def mlstm_chunkwise(q, k, v, i_pre, f_pre):
    b_, h_, s_, dh = q.shape
    L = MLSTM_CHUNK
    nc = s_ // L
    k = k * (dh ** -0.5)
    q_c = q.reshape(b_, h_, nc, L, dh)
    k_c = k.reshape(b_, h_, nc, L, dh)
    v_c = v.reshape(b_, h_, nc, L, dh)
    log_f = jax.nn.log_sigmoid(f_pre).reshape(b_, h_, nc, L)
    log_i = i_pre.reshape(b_, h_, nc, L)
    cum_f = jnp.cumsum(log_f, axis=-1)
    f_tot = cum_f[..., -1]
    a = f_tot[..., None] - cum_f + log_i
    a_max = jnp.max(a, axis=-1)
    w = jnp.exp(a - a_max[..., None])
    C_loc = jnp.einsum('bhcs,bhcsv,bhcsk->bhcvk', w, v_c, k_c)
    n_loc = jnp.einsum('bhcs,bhcsk->bhck', w, k_c)

    def step(carry, inp):
        C, n, m = carry
        C_l, n_l, am, ft = inp
        m_new = jnp.maximum(ft + m, am)
        s_old = jnp.exp(ft + m - m_new)
        s_new = jnp.exp(am - m_new)
        C_n = s_old[..., None, None] * C + s_new[..., None, None] * C_l
        n_n = s_old[..., None] * n + s_new[..., None] * n_l
        return (C_n, n_n, m_new), (C, n, m)

    init = (jnp.zeros((b_, h_, dh, dh), jnp.float32), jnp.zeros((b_, h_, dh), jnp.float32),
            jnp.zeros((b_, h_), jnp.float32))
    xs = (C_loc.transpose(2, 0, 1, 3, 4), n_loc.transpose(2, 0, 1, 3),
          a_max.transpose(2, 0, 1), f_tot.transpose(2, 0, 1))
    _, (C_prev, n_prev, m_prev) = lax.scan(step, init, xs)
    C_prev = C_prev.transpose(1, 2, 0, 3, 4)
    n_prev = n_prev.transpose(1, 2, 0, 3)
    m_prev = m_prev.transpose(1, 2, 0)

    causal = jnp.tril(jnp.ones((L, L), dtype=bool))
    D = cum_f[..., :, None] - cum_f[..., None, :] + log_i[..., None, :]
    D = jnp.where(causal, D, -jnp.inf)
    m_inter = cum_f + m_prev[..., None]
    m_t = jnp.maximum(m_inter, jnp.max(D, axis=-1))
    inter_scale = jnp.exp(m_inter - m_t)
    S_qk = jnp.einsum('bhctd,bhcsd->bhcts', q_c, k_c) * jnp.exp(D - m_t[..., None])
    num = (jnp.einsum('bhcts,bhcsd->bhctd', S_qk, v_c)
           + inter_scale[..., None] * jnp.einsum('bhcvk,bhctk->bhctv', C_prev, q_c))
    den = jnp.sum(S_qk, axis=-1) + inter_scale * jnp.einsum('bhck,bhctk->bhct', n_prev, q_c)
    h = num / jnp.maximum(jnp.abs(den), jnp.exp(-m_t))[..., None]
    return h.reshape(b_, h_, s_, dh)


def moba_attention(q, k, v, slopes):
    b_, h_, s_, dh = q.shape
    blk = MOBA_BLOCK
    nb = -(-s_ // blk)
    s_pad = nb * blk
    k_eff = min(MOBA_TOPK, nb)
    pad = ((0, 0), (0, 0), (0, s_pad - s_), (0, 0))
    k_p = jnp.pad(k, pad)
    v_p = jnp.pad(v, pad)
    k_blk = k_p.reshape(b_, h_, nb, blk, dh)
    v_blk = v_p.reshape(b_, h_, nb, blk, dh)
    k_mean = jnp.mean(k_blk, axis=3)
    scale = dh ** -0.5
    qc_n = MOBA_Q_CHUNK
    n_q = s_ // qc_n
    q_chunks = q.reshape(b_, h_, n_q, qc_n, dh).transpose(2, 0, 1, 3, 4)
    b_idx = jnp.arange(b_)[:, None, None, None]
    h_idx = jnp.arange(h_)[None, :, None, None]
    slope = slopes[None, :, None, None]

    def step(args):
        qc, ci = args
        t0 = ci * qc_n
        pos_q = t0 + jnp.arange(qc_n, dtype=jnp.int32)
        j = t0 // blk
        gate = jnp.einsum('bhqd,bhnd->bhqn', qc, k_mean)
        past = jnp.arange(nb, dtype=jnp.int32) < j
        gate = jnp.where(past, gate, -jnp.inf)
        _, sel = lax.top_k(gate, k_eff)
        sel_valid = jnp.arange(k_eff, dtype=jnp.int32) < j
        k_sel = k_blk[b_idx, h_idx, sel]
        v_sel = v_blk[b_idx, h_idx, sel]
        key_pos = sel[..., None] * blk + jnp.arange(blk, dtype=jnp.int32)
        dist = (pos_q[None, None, :, None, None] - key_pos).astype(jnp.float32)
        sc_sel = jnp.einsum('bhqd,bhqnkd->bhqnk', qc, k_sel) * scale - slope[..., None] * jnp.abs(dist)
        sc_sel = jnp.where(sel_valid[:, None], sc_sel, -jnp.inf)
        k_own = lax.dynamic_slice_in_dim(k_p, j * blk, blk, axis=2)
        v_own = lax.dynamic_slice_in_dim(v_p, j * blk, blk, axis=2)
        own_pos = j * blk + jnp.arange(blk, dtype=jnp.int32)
        dist_own = (pos_q[:, None] - own_pos[None, :])
        sc_own = jnp.einsum('bhqd,bhkd->bhqk', qc, k_own) * scale - slope * jnp.abs(dist_own).astype(jnp.float32)
        sc_own = jnp.where(dist_own >= 0, sc_own, -jnp.inf)
        scores = jnp.concatenate([sc_sel.reshape(b_, h_, qc_n, k_eff * blk), sc_own], axis=-1)
        p = jax.nn.softmax(scores, axis=-1)
        p_sel = p[..., :k_eff * blk].reshape(b_, h_, qc_n, k_eff, blk)
        p_own = p[..., k_eff * blk:]
        return (jnp.einsum('bhqnk,bhqnkd->bhqd', p_sel, v_sel)
                + jnp.einsum('bhqk,bhkd->bhqd', p_own, v_own))

    out = lax.map(step, (q_chunks, jnp.arange(n_q, dtype=jnp.int32)))
    return out.transpose(1, 2, 0, 3, 4).reshape(b_, h_, s_, dh)


def hybrid_layer(x, c, w_ada, b_ada, g_norm, w_in, conv_w, conv_b, b_igate, b_fgate,
                 g_mlstm_head, w_out):
    b_, s_, _ = x.shape
    ada = jax.nn.silu(c.astype(jnp.float32)) @ w_ada.astype(jnp.float32) + b_ada.astype(jnp.float32)
    shift, scale, gate = jnp.split(ada, 3, axis=-1)
    h = rms_norm(x, g_norm) * (1.0 + scale[:, None]) + shift[:, None]
    proj = h @ w_in.astype(jnp.float32)
    (qm, km, vm, om, im, fm, zm, qb, kb, vb, zb) = jnp.split(
        proj, np.cumsum(SPLIT_SIZES)[:-1].tolist(), axis=-1)

    qk = jax.nn.silu(causal_short_conv(jnp.concatenate([qm, km], axis=-1), conv_w, conv_b))
    qm, km = jnp.split(qk, 2, axis=-1)
    i_pre = (im + b_igate.astype(jnp.float32)).transpose(0, 2, 1)
    f_pre = (fm + b_fgate.astype(jnp.float32)).transpose(0, 2, 1)
    hm = mlstm_chunkwise(to_heads(qm, N_HEADS_MLSTM), to_heads(km, N_HEADS_MLSTM),
                         to_heads(vm, N_HEADS_MLSTM), i_pre, f_pre)
    hm = hm * lax.rsqrt(jnp.mean(hm * hm, axis=-1, keepdims=True) + EPS)
    hm = hm.transpose(0, 2, 1, 3).reshape(b_, s_, D_MLSTM) * g_mlstm_head.astype(jnp.float32)
    out_a = hm * jax.nn.sigmoid(om) * jax.nn.silu(zm)

    hb = moba_attention(to_heads(qb, N_HEADS_MOBA), to_heads(kb, N_HEADS_MOBA),
                        to_heads(vb, N_HEADS_MOBA), alibi_slopes(N_HEADS_MOBA))
    out_b = hb.transpose(0, 2, 1, 3).reshape(b_, s_, D_MOBA) * jax.nn.silu(zb)

    y = jnp.concatenate([out_a, out_b], axis=-1) @ w_out.astype(jnp.float32)
    return x.astype(jnp.float32) + gate[:, None] * y


def setup_inputs(seed: int = 0) -> dict:
    key = jax.random.key(seed)
    ks = jax.random.split(key, 13)
    f32 = jnp.float32
    x = jax.random.normal(ks[0], (BATCH, SEQ, D_MODEL), f32)
    c = jax.random.normal(ks[1], (BATCH, D_MODEL), f32)
    w_ada = jax.random.normal(ks[2], (DEPTH, D_MODEL, 3 * D_MODEL), f32) * D_MODEL ** -0.5
    b_ada = 0.01 * jax.random.normal(ks[3], (DEPTH, 3 * D_MODEL), f32)
    g_norm = 1.0 + 0.02 * jax.random.normal(ks[4], (DEPTH, D_MODEL), f32)
    w_in = jax.random.normal(ks[5], (DEPTH, D_MODEL, PROJ_DIM), f32) * D_MODEL ** -0.5
    conv_w = jax.random.normal(ks[6], (DEPTH, CONV_K, 2 * D_MLSTM), f32) * CONV_K ** -0.5
    conv_b = 0.01 * jax.random.normal(ks[7], (DEPTH, 2 * D_MLSTM), f32)
    b_igate = 0.1 * jax.random.normal(ks[8], (DEPTH, N_HEADS_MLSTM), f32)
    b_fgate = (jnp.linspace(3.0, 6.0, N_HEADS_MLSTM, dtype=f32)[None, :]
               + 0.1 * jax.random.normal(ks[9], (DEPTH, N_HEADS_MLSTM), f32))
    g_mlstm_head = 1.0 + 0.02 * jax.random.normal(ks[10], (DEPTH, D_MLSTM), f32)
    w_out = jax.random.normal(ks[11], (DEPTH, D_MIX, D_MODEL), f32) * D_MIX ** -0.5
    g_final = 1.0 + 0.02 * jax.random.normal(ks[12], (D_MODEL,), f32)
    return {"x": x, "c": c, "w_ada": w_ada, "b_ada": b_ada, "g_norm": g_norm, "w_in": w_in,
            "conv_w": conv_w, "conv_b": conv_b, "b_igate": b_igate, "b_fgate": b_fgate,
            "g_mlstm_head": g_mlstm_head, "w_out": w_out, "g_final": g_final}


def reference(x, c, w_ada, b_ada, g_norm, w_in, conv_w, conv_b, b_igate, b_fgate,
              g_mlstm_head, w_out, g_final):
    h = x.astype(jnp.float32)
    for layer in range(DEPTH):
        h = hybrid_layer(h, c, w_ada[layer], b_ada[layer], g_norm[layer], w_in[layer],
                         conv_w[layer], conv_b[layer], b_igate[layer], b_fgate[layer],
                         g_mlstm_head[layer], w_out[layer])
    return rms_norm(h, g_final).astype(x.dtype)
```

```python
import numpy as np
import ml_dtypes
import concourse.bass as bass
import concourse.mybir as mybir
from concourse.bass_utils import run_bass_kernel_spmd

F32 = mybir.dt.float32
BF16 = mybir.dt.bfloat16
AF = mybir.ActivationFunctionType
ALU = mybir.AluOpType

_COMPUTE = ('pe', 'act', 'dve', 'pool')


class Prog:
    def __init__(self, nc):
        self.nc = nc
        self.ops = []
        self.last_w = {}
        self.readers = {}
        self.eng_sem = {}
        self.key_sem = {}
        self.final_keys = []
        self.bar = None

    def barrier(self, exclude=()):
        lasts = {}
        for i, o in enumerate(self.ops):
            key = ('k', o['semkey']) if o['dma'] else ('e', o['eng'])
            if o['dma'] and o['semkey'] in exclude:
                continue
            lasts[key] = i
        self.bar = (set(lasts.values()), set())

    def fence(self, dep_ops):
        self.bar = (set(dep_ops), set())

    def _add(self, eng, fn, reads, writes, is_dma, semkey=None, extra=()):
        i = len(self.ops)
        deps = set(extra)
        psr = [k for k in reads if isinstance(k, str) and k.startswith('ps')]
        if psr:
            reads = [k for k in reads if k not in psr]
            writes = list(writes) + psr
        if self.bar is not None and eng not in self.bar[1]:
            deps |= self.bar[0]
            self.bar[1].add(eng)
        for k in reads:
            if k in self.last_w:
                deps.add(self.last_w[k])
        for k in writes:
            if k in self.last_w:
                deps.add(self.last_w[k])
            for r in self.readers.get(k, ()):
                deps.add(r)
        deps.discard(i)
        self.ops.append(dict(eng=eng, fn=fn, deps=sorted(deps), dma=is_dma, semkey=semkey, sig=is_dma))
        for k in reads:
            self.readers.setdefault(k, []).append(i)
        for k in writes:
            self.last_w[k] = i
            self.readers[k] = []
        return i

    def op(self, eng, fn, reads=(), writes=()):
        return self._add(eng, fn, list(reads), list(writes), False)

    def dma(self, queue, out, in_, reads=(), writes=(), final=False, group=None, extra=()):
        writes = list(writes)
        semkey = writes[0] if group is None else ('G', group)
        if final:
            self.final_keys.append(semkey)
        return self._add(queue, lambda e: e.dma_start(out=out, in_=in_), list(reads), writes, True, semkey, extra=extra)

    def emit(self):
        nc = self.nc
        ops = self.ops
        for o in ops:
            for d in o['deps']:
                p = ops[d]
                if p['dma']:
                    continue
                if p['eng'] == 'pe' and o['eng'] == 'pe' and not o['dma']:
                    continue
                p['sig'] = True
        cnt = {}
        for o in ops:
            if o['dma']:
                k = ('k', o['semkey'])
                cnt[k] = cnt.get(k, 0) + 16
                o['tok'] = (k, cnt[k])
            elif o['sig']:
                k = ('e', o['eng'])
                cnt[k] = cnt.get(k, 0) + 1
                o['tok'] = (k, cnt[k])
            else:
                o['tok'] = None
        sems = {}
        for k in cnt:
            sems[k] = nc.alloc_semaphore("s_%s_%s" % (k[0], str(k[1]).replace(' ', '_')))
        self.sems = sems
        final_tok = {}
        for o in ops:
            if o['dma'] and o['semkey'] in self.final_keys:
                final_tok[o['tok'][0]] = o['tok'][1]
        seen = {e: {} for e in ('pe', 'act', 'dve', 'pool', 'sp')}
        per_eng = {e: [] for e in seen}
        for o in ops:
            e = o['eng']
            waits = []
            for d in o['deps']:
                p = ops[d]
                if (not p['dma']) and (not o['dma']) and p['eng'] == 'pe' and e == 'pe':
                    continue
                k, v = p['tok']
                if p['dma'] and isinstance(p['semkey'], tuple):
                    v = max(q['tok'][1] for q in ops[:ops.index(o)] if q['dma'] and q['semkey'] == p['semkey'])
                if seen[e].get(k, 0) >= v:
                    continue
                seen[e][k] = v
                waits.append((k, v))
            per_eng[e].append((o, waits))

        def run(engname, handle):
            for o, waits in per_eng[engname]:
                for k, v in waits:
                    handle.wait_ge(sems[k], v)
                ins = o['fn'](handle)
                if o['tok'] is not None:
                    k, v = o['tok']
                    ins.then_inc(sems[k], 16 if o['dma'] else 1)
            if engname == 'sp':
                for k, v in final_tok.items():
                    handle.wait_ge(sems[k], v)

        with nc.Block() as block:
            @block.tensor
            def _(t):
                run('pe', t)

            @block.scalar
            def _(s):
                run('act', s)

            @block.vector
            def _(v):
                run('dve', v)

            @block.gpsimd
            def _(g):
                run('pool', g)

            @block.sync
            def _(sy):
                run('sp', sy)
        self.ops = []


S = 2048
D = 1024
NT = 16
PROJ = 4616
EPS = 1e-6
C_QM, C_KM, C_VM, C_OM, C_IF, C_ZM, C_QB, C_KB, C_VB, C_ZB = 0, 512, 1024, 1536, 2048, 2056, 2568, 3080, 3592, 4104
LN_S = float(np.log(128.0 ** -0.5))
NEG = -1.0e30


class Arena:
    def __init__(self, nc, limit=16384 + 207 * 1024):
        self.nc = nc
        self.off = 16384
        self.limit = limit
        self.n = 0

    def alloc(self, name, shape, dtype):
        esz = 4 if dtype == F32 else 2
        per_part = int(np.prod(shape[1:])) * esz
        self.off = (self.off + 63) // 64 * 64
        t = self.nc.alloc_sbuf_tensor_at("%s_%d" % (name, self.n), list(shape), dtype, offset=self.off)
        self.n += 1
        self.off += per_part
        assert self.off <= self.limit, (name, self.off)
        return t

    def mark(self):
        return self.off

    def release(self, m):
        self.off = m


def build_program(upto=99, dumps=()):
    nc = bass.Bass("TRN2", target_bir_lowering=False)
    P = Prog(nc)
    A = Arena(nc)

    def din(name, shape, dt=F32):
        return nc.dram_tensor(name, list(shape), dt, kind="ExternalInput")

    x_d = din("x", [S, D])
    cT_d = din("cT", [128, 8])
    wada_d = din("w_ada", [D, 3 * D])
    bss_d = din("bss", [128, 16])
    bgate_d = din("bgate", [128, D])
    gnT_d = din("gnT", [128, 8])
    win_d = din("w_in", [D, PROJ])
    convw_d = din("convw", [128, 32])
    convb_d = din("convb", [128, 8])
    bif_d = din("bif", [128, 128])
    gml_d = din("gml", [128, 4])
    wout_d = din("w_out", [D, D])
    gfin_d = din("gfin", [128, D])
    identb_d = din("identb", [128, 128], BF16)
    identf_d = din("identf", [128, 128])
    U_d = din("U", [128, 128])
    tri_d = din("tri01", [128, 128], BF16)
    cmask_d = din("cmask", [128, 128], BF16)
    kaug_d = din("kaugc", [11, 8, S], BF16)
    qaug_d = din("qaugc", [3, 8, S], BF16)
    sel_d = din("selc", [8, 8, 64], BF16)
    out_d = nc.dram_tensor("out", [S, D], F32, kind="ExternalOutput")
    scr_d = nc.dram_tensor("scr", [8, 512], F32, kind="Internal")
    dump_d = {}

    PS = nc.alloc_psum_tensor("ps", [128, 8, 512], F32)
    PSB = PS.bitcast(BF16)

    identb = A.alloc("identb", [128, 128], BF16)
    identf = A.alloc("identf", [128, 128], F32)
    U = A.alloc("U", [128, 128], F32)
    onesf = A.alloc("onesf", [128, 128], F32)
    tri01 = A.alloc("tri01", [128, 128], BF16)
    cmask = A.alloc("cmask", [128, 128], BF16)
    gs = A.alloc("gs", [128, 8], F32)
    shiftT = A.alloc("shiftT", [128, 8], F32)
    gate_bc = A.alloc("gate_bc", [128, D], F32)
    convw = A.alloc("convw", [128, 32], F32)
    convb = A.alloc("convb", [128, 8], F32)
    bif = A.alloc("bif", [128, 128], F32)
    gml = A.alloc("gml", [128, 4], F32)
    A.off = (A.off + 63) // 64 * 64
    catT_off = A.off
    catT = A.alloc("catT", [128, 8, S], BF16)
    wbufs = [A.alloc("wbuf%d" % i, [128, 8, 512], BF16) for i in range(2)]
    screp = A.alloc("screp", [128, 8, 128], BF16)
    A.off = (A.off + 63) // 64 * 64
    hT_off = A.off
    hT = A.alloc("hT", [128, 8, S], BF16)
    base_mark = A.mark()

    def ld(dst, src, key, q='sp', group='consts'):
        return P.dma(q, dst, src, writes=[key], group=group)

    win_v = win_d.rearrange("(kc p) c -> p kc c", p=128)
    pre_w = [(wbufs[grp], 'wbuf%d' % grp) for grp in range(2)]

    ld(identb[:, :], identb_d[:, :], 'identb')
    ld(identf[:, :], identf_d[:, :], 'identf')
    ld(U[:, :], U_d[:, :], 'U')
    ld(tri01[:, :], tri_d[:, :], 'tri01')
    ld(cmask[:, :], cmask_d[:, :], 'cmask')
    ld(convw[:, :], convw_d[:, :], 'convw')
    ld(convb[:, :], convb_d[:, :], 'convb')
    ld(bif[:, :], bif_d[:, :], 'bif')
    ld(gml[:, :], gml_d[:, :], 'gml')
    P.op('pool', lambda e: e.memset(onesf[:, :], 1.0), writes=['onesf'])

    def add_dump(name, ap, shape, dt, key):
        d = nc.dram_tensor("dbg_" + name, list(shape), dt, kind="ExternalOutput")
        dump_d[name] = d
        P.dma('sp', d[tuple(slice(None) for _ in shape)], ap, reads=(key if isinstance(key, list) else [key]), writes=['dbg_' + name], final=True)

    m = A.mark()
    xall = A.alloc("xall", [128, NT, D], F32)
    _sv = A.off
    A.off = catT_off
    xn = A.alloc("xn", [128, NT, D], BF16)
    A.off = _sv
    junks = [A.alloc("junk%d" % i, [128, D], BF16) for i in range(2)]
    wst = [A.alloc("wst%d" % i, [128, 2 * D], BF16) for i in range(2)]
    wsf = [A.alloc("wsf%d" % i, [128, 2 * D], F32) for i in range(2)]
    sc = A.alloc("sc", [128, 8], BF16)
    cT = A.alloc("cT", [128, 8], F32)
    ssq = A.alloc("ssq", [128, NT], F32)
    rstd = A.alloc("rstd", [128, NT], F32)
    bss = A.alloc("bss", [128, 16], F32)
    gnT = A.alloc("gnT", [128, 8], F32)
    scl1 = A.alloc("scl1", [128, 8], F32)

    ld(cT[:, :], cT_d[:, :], 'cT')
    ld(bss[:, :], bss_d[:, :], 'bss')
    ld(gnT[:, :], gnT_d[:, :], 'gnT')
    ld(gate_bc[:, :], bgate_d[:, :], 'gate_bc')
    P.op('act', lambda e: e.activation(out=sc[:, :], in_=cT[:, :], func=AF.Silu), reads=['cT'], writes=['sc'])
    for kc in range(8):
        P.op('dve', lambda e, kc=kc: e.tensor_scalar(out=screp[:, kc, :], in0=onesf[:, :], scalar1=sc[:, kc:kc + 1], scalar2=None, op0=ALU.mult),
             reads=['onesf', 'sc'], writes=['screp%d' % kc])
    tcnt = [0]

    def x_group(g, modulate=False):
        for t in range(4 * g, 4 * g + 4):
            P.op('act', lambda e, t=t: e.activation(out=junks[t % 2][:, :], in_=xall[:, t, :], func=AF.Square, accum_out=ssq[:, t:t + 1]),
                 reads=['xall%d' % t], writes=['ssq%d' % t, 'junk%d' % (t % 2)])
        P.op('act', lambda e: e.activation(out=rstd[:, 4 * g:4 * g + 4], in_=ssq[:, 4 * g:4 * g + 4], func=AF.Sqrt, scale=1.0 / D, bias=EPS),
             reads=['ssq%d' % t for t in range(4 * g, 4 * g + 4)], writes=['rstd%d' % g])
        P.op('dve', lambda e: e.reciprocal(rstd[:, 4 * g:4 * g + 4], rstd[:, 4 * g:4 * g + 4]), reads=['rstd%d' % g], writes=['rstd%d' % g])
        for t in range(4 * g, 4 * g + 4):
            P.op('dve', lambda e, t=t: e.tensor_scalar(out=xn[:, t, :], in0=xall[:, t, :], scalar1=rstd[:, t:t + 1], scalar2=None, op0=ALU.mult),
                 reads=['xall%d' % t, 'rstd%d' % g], writes=['xn%d' % t])
        for dc in range(8):
            b = 3 + (tcnt[0] % 4)
            for j in range(4):
                t = 4 * g + j
                P.op('pe', lambda e, b=b, j=j, t=t, dc=dc: e.transpose(PSB[:, b, j * 128:(j + 1) * 128], xn[:, t, dc * 128:(dc + 1) * 128], identb[:, :]),
                     reads=['xn%d' % t, 'identb'], writes=['ps%d' % b])
            if modulate:
                if tcnt[0] % 2 == 0:
                    P.op('act', lambda e, b=b, dc=dc: e.activation(out=hT[:, dc, g * 512:(g + 1) * 512], in_=PSB[:, b, 0:512], func=AF.Identity,
                                                                   scale=gs[:, dc:dc + 1], bias=shiftT[:, dc:dc + 1]),
                         reads=['ps%d' % b, 'gs', 'shiftT'], writes=['hT%d_%d' % (dc, g)])
                else:
                    P.op('dve', lambda e, b=b, dc=dc: e.tensor_scalar(out=hT[:, dc, g * 512:(g + 1) * 512], in0=PSB[:, b, 0:512],
                                                                       scalar1=gs[:, dc:dc + 1], scalar2=shiftT[:, dc:dc + 1], op0=ALU.mult, op1=ALU.add),
                         reads=['ps%d' % b, 'gs', 'shiftT'], writes=['hT%d_%d' % (dc, g)])
            elif tcnt[0] % 2 == 0:
                P.op('act', lambda e, b=b, dc=dc: e.activation(out=hT[:, dc, g * 512:(g + 1) * 512], in_=PSB[:, b, 0:512], func=AF.Copy),
                     reads=['ps%d' % b], writes=['hT%d_%d' % (dc, g)])
            else:
                P.op('dve', lambda e, b=b, dc=dc: e.tensor_copy(hT[:, dc, g * 512:(g + 1) * 512], PSB[:, b, 0:512]),
                     reads=['ps%d' % b], writes=['hT%d_%d' % (dc, g)])
            tcnt[0] += 1

    sfi = 0
    for kc in range(8):
        w = wst[kc % 2]
        wk = 'wst%d' % (kc % 2)
        wf = wsf[sfi % 2]
        wfk = 'wsf%d' % (sfi % 2)
        sfi += 1
        P.dma('act', wf[:, :], wada_d[kc * 128:(kc + 1) * 128, 0:2048], writes=[wfk])
        P.op('dve', lambda e, w=w, wf=wf: e.tensor_copy(w[:, :], wf[:, :]), reads=[wfk], writes=[wk])
        for t in (2 * kc, 2 * kc + 1):
            last_x_ld = ld(xall[:, t, :], x_d[t * 128:(t + 1) * 128, :], 'xall%d' % t, group='xall%d' % (t // 4))
        for j in range(16):
            P.op('pe', lambda e, w=w, j=j, kc=kc: e.matmul(PS[:, 0, j:j + 1], w[:, j * 128:(j + 1) * 128], sc[:, kc:kc + 1],
                                                            start=(kc == 0 and j == 0), stop=(kc == 7), skip_group_check=True),
                 reads=[wk, 'sc'], writes=['ps0'])
        if kc == 7:
            for grp in range(2):
                P.dma('pool', wbufs[grp][:, :, 0:512], win_v[:, :, C_QM + grp * 512:C_QM + (grp + 1) * 512], writes=['wbuf%d' % grp],
                      extra=[last_x_ld])
        if kc % 2 == 1 and kc >= 3:
            x_group((kc - 3) // 2)
    P.op('dve', lambda e: e.tensor_tensor(out=shiftT[:, :], in0=PS[:, 0, 0:8], in1=bss[:, 0:8], op=ALU.add), reads=['ps0', 'bss'], writes=['shiftT'])
    P.op('dve', lambda e: e.scalar_tensor_tensor(out=scl1[:, :], in0=PS[:, 0, 8:16], scalar=1.0, in1=bss[:, 8:16], op0=ALU.add, op1=ALU.add),
         reads=['ps0', 'bss'], writes=['scl1'])
    P.op('dve', lambda e: e.tensor_tensor(out=gs[:, :], in0=scl1[:, :], in1=gnT[:, :], op=ALU.mult), reads=['scl1', 'gnT'], writes=['gs'])
    for dc in range(8):
        eng = 'dve' if dc % 2 == 0 else 'pool'
        P.op(eng, lambda e, dc=dc: e.tensor_scalar(out=hT[:, dc, 0:1536], in0=hT[:, dc, 0:1536], scalar1=gs[:, dc:dc + 1], scalar2=shiftT[:, dc:dc + 1],
                                                   op0=ALU.mult, op1=ALU.add),
             reads=['hT%d_%d' % (dc, g) for g in range(3)] + ['gs', 'shiftT'], writes=['hT%d_%d' % (dc, g) for g in range(3)])
    x_group(3, modulate=True)
    if 'hT' in dumps:
        add_dump('hT', hT[:, :, :], [128, 8, S], BF16, ['hT%d_%d' % (a, b) for a in range(8) for b in range(4)])
    if 'gate_bc' in dumps:
        add_dump('gate_bc', gate_bc[:, :], [128, D], F32, 'gate_bc')
    A.release(m)
    if upto <= 1:
        P.emit()
        return nc, dump_d

    P.barrier()
    PS4 = PS.reshape([128, 8, 4, 128])
    wslot = [0]

    def load_w(c0, ncols):
        sl = wslot[0] % 2
        wslot[0] += 1
        P.dma('pool', wbufs[sl][:, :, 0:ncols], win_v[:, :, c0:c0 + ncols], writes=['wbuf%d' % sl])
        return wbufs[sl], 'wbuf%d' % sl

    bank_rr = [0]

    def next_bank(lo=0, n=4):
        b = lo + bank_rr[0] % n
        bank_rr[0] += 1
        return b

    evac_rr = [0]

    def evac_copy(out, in_, reads, writes, scale=None, force_act=False):
        evac_rr[0] += 1
        if force_act or evac_rr[0] % 2 == 0:
            if scale is None:
                P.op('act', lambda e: e.activation(out=out, in_=in_, func=AF.Copy), reads=reads, writes=writes)
            else:
                P.op('act', lambda e: e.activation(out=out, in_=in_, func=AF.Copy, scale=scale), reads=reads, writes=writes)
        else:
            if scale is None:
                P.op('dve', lambda e: e.tensor_copy(out, in_), reads=reads, writes=writes)
            else:
                P.op('dve', lambda e: e.tensor_scalar(out=out, in0=in_, scalar1=scale, scalar2=None, op0=ALU.mult), reads=reads, writes=writes)

    def hkeys(g):
        return ['hT%d_%d' % (kc, g) for kc in range(8)]

    def proj_fm(w, wk, cl, g, b):
        for kc in range(8):
            P.op('pe', lambda e, kc=kc: e.matmul(PS[:, b, :], w[:, kc, cl * 128:(cl + 1) * 128], hT[:, kc, g * 512:(g + 1) * 512],
                                                  start=(kc == 0), stop=(kc == 7)),
                 reads=[wk] + hkeys(g), writes=['ps%d' % b])

    def proj_tm(w, wk, t, ncols, out_ap, outkey):
        for kc in range(8):
            P.op('pe', lambda e, kc=kc: e.matmul(out_ap, hT[:, kc, t * 128:(t + 1) * 128], w[:, kc, 0:ncols],
                                                  start=(kc == 0), stop=(kc == 7)),
                 reads=[wk] + hkeys(t // 4), writes=[outkey])

    mB = A.mark()
    qkT = A.alloc("qkT", [128, 8, S], BF16)
    vaug = A.alloc("vaug", [128, NT, 4, 130], BF16)
    A.off = (A.off + 63) // 64 * 64
    gT_off = A.off
    gT = A.alloc("gT", [128, 4, S], BF16)
    _sv2 = A.off
    A.off = gT_off
    wg = A.alloc("wg", [128, 8, D], BF16)
    A.off = _sv2
    u12 = A.alloc("u12", [128, 4, 64], F32)
    mB2 = A.mark()
    raw = A.alloc("raw", [128, 8, S + 4], BF16)
    cacc = [A.alloc("cacc%d" % i, [128, 512], F32) for i in range(4)]
    wif = A.alloc("wif", [128, 8, 8], BF16)
    gtmp = [A.alloc("gtmp%d" % i, [128, 512], F32) for i in range(2)]
    szt = [A.alloc("szt%d" % i, [128, 512], BF16) for i in range(2)]
    fi = A.alloc("fi", [128, 128], F32)
    nl = A.alloc("nl", [128, 64], F32)
    linc = A.alloc("linc", [128, 68], F32)
    NF = A.alloc("NF", [128, 64], F32)
    gg = A.alloc("gg", [128, 64], F32)
    tmax = A.alloc("tmax", [128, 1], F32)
    trow = A.alloc("trow", [1, 64], F32)
    Rrow = A.alloc("Rrow", [1, 68], F32)
    Rb = A.alloc("Rb", [128, 68], F32)
    fi3 = fi.reshape([128, 16, 8])
    nl3 = nl.reshape([128, 16, 4])
    NF3 = NF.reshape([128, 16, 4])
    gg3 = gg.reshape([128, 16, 4])

    P.dma('pool', wif[:, :, :], win_v[:, :, C_IF:C_IF + 8], writes=['wif'])
    for k2 in range(8):
        P.dma('pool', wg[:, k2, :], wada_d[k2 * 128:(k2 + 1) * 128, 2048:3072], writes=['wg%d' % k2], group='wg')
    P.op('pool', lambda e: e.memset(raw[:, :, 0:3], 0.0), writes=['rawpad'])
    P.op('pool', lambda e: e.memset(vaug[:, :, :, 128:129], 1.0), writes=['vones'])
    def conv_tile(ct):
        for gp in range(2):
            gs_ = (2 * gp, 2 * gp + 1)
            for g in gs_:
                acc = cacc[g]
                P.op('dve', lambda e, g=g, acc=acc: e.tensor_scalar(out=acc[:, :], in0=raw[:, ct, g * 512 + 3:g * 512 + 3 + 512], scalar1=convw[:, ct * 4 + 3:ct * 4 + 4],
                                                                    scalar2=convb[:, ct:ct + 1], op0=ALU.mult, op1=ALU.add),
                     reads=['raw%d_%d' % (ct, g), 'convw', 'convb'], writes=['cacc%d' % g])
            for tap in range(3):
                for g in gs_:
                    acc = cacc[g]
                    last = (tap == 2)
                    out_ap = qkT[:, ct, g * 512:(g + 1) * 512] if last else acc[:, :]
                    P.op('dve', lambda e, g=g, acc=acc, tap=tap, out_ap=out_ap: e.scalar_tensor_tensor(out=out_ap, in0=raw[:, ct, g * 512 + tap:g * 512 + tap + 512],
                                                                                                       scalar=convw[:, ct * 4 + tap:ct * 4 + tap + 1], in1=acc[:, :],
                                                                                                       op0=ALU.mult, op1=ALU.add),
                         reads=['raw%d_%d' % (ct, g), 'rawpad', 'convw', 'cacc%d' % g] + (['raw%d_%d' % (ct, g - 1)] if g > 0 else []),
                         writes=(['qkTpre%d_%d' % (ct, g)] if last else ['cacc%d' % g]))

    def conv_silu(ct):
        P.op('act', lambda e: e.activation(out=qkT[:, ct, :], in_=qkT[:, ct, :], func=AF.Silu),
             reads=['qkTpre%d_%d' % (ct, g) for g in range(4)], writes=['qkT%d_%d' % (ct, g) for g in range(4)])

    for grp in range(2):
        w, wk = pre_w[grp]
        for cl in range(4):
            ct = grp * 4 + cl
            for g in range(4):
                b = next_bank(0, 4)
                proj_fm(w, wk, cl, g, b)
                evac_copy(raw[:, ct, 3 + g * 512:3 + (g + 1) * 512], PS[:, b, :], ['ps%d' % b], ['raw%d_%d' % (ct, g)], force_act=True)
            if ct >= 1:
                conv_tile(ct - 1)
    w, wk = load_w(C_VM, 512)
    for t in range(NT):
        b = next_bank(0, 4)
        proj_tm(w, wk, t, 512, PS[:, b, :], 'ps%d' % b)
        evac_copy(vaug[:, t, :, 0:128], PS4[:, b, :, :], ['ps%d' % b], ['vaug%d' % t], force_act=True)
    conv_tile(7)
    for t in range(NT):
        proj_tm(wif, 'wif', t, 8, PS[:, 6, t * 8:(t + 1) * 8], 'ps6')
    P.op('dve', lambda e: e.tensor_tensor(out=fi[:, :], in0=PS[:, 6, 0:128], in1=bif[:, :], op=ALU.add), reads=['ps6', 'bif'], writes=['fi'])
    for n in range(2):
        b = next_bank(0, 4)
        for kc in range(8):
            P.op('pe', lambda e, n=n, kc=kc, b=b: e.matmul(PS[:, b, :], screp[:, kc, :], wg[:, kc, n * 512:(n + 1) * 512], start=(kc == 0), stop=(kc == 7)),
                 reads=['wg%d' % kc, 'screp%d' % kc], writes=['ps%d' % b])
        P.op('dve', lambda e, n=n, b=b: e.tensor_tensor(out=gate_bc[:, n * 512:(n + 1) * 512], in0=PS[:, b, :], in1=gate_bc[:, n * 512:(n + 1) * 512], op=ALU.add),
             reads=['ps%d' % b, 'gate_bc'], writes=['gate_bc', 'gate_done%d' % n])
    w_o, wk_o = load_w(C_OM, 512)
    w_z, wk_z = load_w(C_ZM, 512)
    ozg = []
    zi_c = [0]

    def mk_o(cl, g, idx):
        def f():
            if idx % 4 == 1:
                conv_silu(idx // 4)
            b = next_bank(0, 4)
            proj_fm(w_o, wk_o, cl, g, b)
            P.op('act', lambda e: e.activation(out=gT[:, cl, g * 512:(g + 1) * 512], in_=PS[:, b, :], func=AF.Sigmoid),
                 reads=['ps%d' % b, 'gate_done0', 'gate_done1'], writes=['gT%d_%d' % (cl, g)])
        return f

    def mk_z(cl, g, idx):
        def f():
            if idx % 4 == 1:
                conv_silu(4 + idx // 4)
            b = next_bank(0, 4)
            proj_fm(w_z, wk_z, cl, g, b)
            sz = szt[zi_c[0] % 2]
            szk = 'szt%d' % (zi_c[0] % 2)
            zi_c[0] += 1
            P.op('act', lambda e: e.activation(out=sz[:, :], in_=PS[:, b, :], func=AF.Silu), reads=['ps%d' % b], writes=[szk])
            P.op('dve', lambda e: e.scalar_tensor_tensor(out=gT[:, cl, g * 512:(g + 1) * 512], in0=gT[:, cl, g * 512:(g + 1) * 512],
                                                         scalar=gml[:, cl:cl + 1], in1=sz[:, :], op0=ALU.mult, op1=ALU.mult),
                 reads=['gT%d_%d' % (cl, g), szk, 'gml'], writes=['gT%d_%d' % (cl, g)])
        return f

    for cl in range(4):
        for g in range(4):
            ozg.append(mk_o(cl, g, cl * 4 + g))
    for cl in range(4):
        for g in range(4):
            ozg.append(mk_z(cl, g, cl * 4 + g))

    def emit_oz(n):
        for _ in range(n):
            if ozg:
                ozg.pop(0)()

    P.op('act', lambda e: e.activation(out=nl3[:, :, :], in_=fi3[:, :, 4:8], func=AF.Exp, scale=-1.0), reads=['fi'], writes=['nl'])
    P.op('act', lambda e: e.activation(out=nl[:, :], in_=nl[:, :], func=AF.Ln, bias=1.0), reads=['nl'], writes=['nl'])
    P.op('dve', lambda e: e.memset(linc[:, 0:4], 0.0), writes=['linc'])
    for t in range(16):
        P.op('dve', lambda e, t=t: e.tensor_tensor(out=linc[:, (t + 1) * 4:(t + 2) * 4], in0=linc[:, t * 4:(t + 1) * 4], in1=nl[:, t * 4:(t + 1) * 4], op=ALU.add),
             reads=['linc', 'nl'], writes=['linc'])
    emit_oz(5)
    P.op('pe', lambda e: e.matmul(PS[:, 7, 0:64], U[:, :], nl[:, :], start=True, stop=False), reads=['U', 'nl'], writes=['ps7'])
    P.op('pe', lambda e: e.matmul(PS[:, 7, 0:64], onesf[:, :], linc[:, 0:64], start=False, stop=True), reads=['onesf', 'linc'], writes=['ps7'])
    P.op('dve', lambda e: e.tensor_copy(NF[:, :], PS[:, 7, 0:64]), reads=['ps7'], writes=['NF'])
    P.op('dve', lambda e: e.tensor_tensor(out=gg3[:, :, :], in0=fi3[:, :, 0:4], in1=NF3[:, :, :], op=ALU.add), reads=['fi', 'NF'], writes=['gg'])
    emit_oz(3)
    P.op('pe', lambda e: e.transpose(PS[0:64, 7, 128:256], gg[:, 0:64], identf[:, :]), reads=['gg', 'identf'], writes=['ps7b'])
    P.op('dve', lambda e: e.tensor_reduce(out=tmax[0:64, 0:1], in_=PS[0:64, 7, 128:256], op=ALU.max, axis=mybir.AxisListType.X), reads=['ps7b'], writes=['tmax'])
    emit_oz(3)
    P.op('pe', lambda e: e.transpose(PS[0:1, 7, 256:320], tmax[0:64, 0:1], identf[0:64, 0:64]), reads=['tmax', 'identf'], writes=['ps7c'])
    P.op('dve', lambda e: e.tensor_copy(trow[0:1, :], PS[0:1, 7, 256:320]), reads=['ps7c'], writes=['trow'])
    P.op('dve', lambda e: e.memset(Rrow[0:1, 0:4], 0.0), writes=['Rrow'])
    for t in range(16):
        P.op('dve', lambda e, t=t: e.tensor_tensor(out=Rrow[0:1, (t + 1) * 4:(t + 2) * 4], in0=Rrow[0:1, t * 4:(t + 1) * 4], in1=trow[0:1, t * 4:(t + 1) * 4], op=ALU.max),
             reads=['Rrow', 'trow'], writes=['Rrow'])
    emit_oz(6)
    P.op('pe', lambda e: e.matmul(PS[:, 7, 320:388], onesf[0:1, 0:128], Rrow[0:1, 0:68], start=True, stop=True), reads=['onesf', 'Rrow'], writes=['ps7d'])
    emit_oz(4)
    P.op('dve', lambda e: e.tensor_copy(Rb[:, :], PS[:, 7, 320:388]), reads=['ps7d'], writes=['Rb'])
    P.op('dve', lambda e: e.tensor_tensor(out=u12[:, 0, :], in0=gg[:, :], in1=Rb[:, 0:64], op=ALU.subtract), reads=['gg', 'Rb'], writes=['u12'])
    P.op('dve', lambda e: e.tensor_tensor(out=u12[:, 1, :], in0=gg[:, :], in1=Rb[:, 4:68], op=ALU.subtract), reads=['gg', 'Rb'], writes=['u12'])
    P.op('dve', lambda e: e.tensor_tensor(out=u12[:, 2, :], in0=NF[:, :], in1=Rb[:, 0:64], op=ALU.subtract), reads=['NF', 'Rb'], writes=['u12'])
    P.op('dve', lambda e: e.tensor_tensor(out=u12[:, 3, :], in0=Rb[:, 0:64], in1=Rb[:, 4:68], op=ALU.subtract), reads=['Rb'], writes=['u12'])
    P.op('act', lambda e: e.activation(out=u12[:, 0:2, :], in_=u12[:, 0:2, :], func=AF.Exp, bias=LN_S), reads=['u12'], writes=['u12'])
    P.op('act', lambda e: e.activation(out=u12[:, 2:4, :], in_=u12[:, 2:4, :], func=AF.Exp), reads=['u12'], writes=['u12'])
    emit_oz(len(ozg))
    preC = [load_w(C_QB, 512), load_w(C_KB, 512)]
    if 'qkT' in dumps:
        add_dump('qkT', qkT[:, :, :], [128, 8, S], BF16, ['qkT%d_%d' % (a, b) for a in range(8) for b in range(4)])
    if 'u12' in dumps:
        add_dump('u12', u12[:, :, :], [128, 4, 64], F32, 'u12')
    if 'gT' in dumps:
        add_dump('gT', gT[:, :, :], [128, 4, S], BF16, ['gT%d_%d' % (a, b) for a in range(4) for b in range(4)])
    A.release(mB2)
    if upto <= 2:
        P.emit()
        return nc, dump_d

    P.barrier(exclude=('wbuf0', 'wbuf1'))
    stil = [A.alloc("stil%d" % i, [128, 128], BF16) for i in range(2)]
    ktil = [A.alloc("ktil%d" % i, [128, 128], BF16) for i in range(2)]
    Cf = [A.alloc("Cf%d" % i, [128, 130], F32) for i in range(4)]
    Cb = [A.alloc("Cb%d" % i, [128, 130], BF16) for i in range(4)]
    Asb = A.alloc("Asb", [128, NT, 4, 130], F32)
    Bsb = A.alloc("Bsb", [128, 64], F32)
    ssqA = A.alloc("ssqA", [128, 64], F32)
    junkA = [A.alloc("junkA%d" % i, [128, 128], BF16) for i in range(2)]
    Af = [A.alloc("Af%d" % i, [128, 130], F32) for i in range(2)]
    sm = A.alloc("sm", [128, 4, 64], F32)
    hnb = [A.alloc("hnb%d" % i, [128, 128], BF16) for i in range(4)]
    stepsB = [(t, h) for t in range(NT) for h in range(4)]

    def stage1(i):
        t, h = stepsB[i]
        par = i % 2
        col = t * 4 + h
        tl = slice(t * 128, (t + 1) * 128)
        qk_q = 'qkT%d_%d' % (h, t // 4)
        qk_k = 'qkT%d_%d' % (4 + h, t // 4)
        P.op('pe', lambda e: e.matmul(PS[:, par, 0:128], qkT[:, 4 + h, tl], qkT[:, h, tl], start=True, stop=True),
             reads=[qk_q, qk_k], writes=['ps%d' % par])
        P.op('dve', lambda e: e.scalar_tensor_tensor(out=stil[par][:, :], in0=PS[:, par, 0:128], scalar=u12[:, 0, col:col + 1], in1=tri01[:, :],
                                                     op0=ALU.mult, op1=ALU.mult),
             reads=['ps%d' % par, 'u12', 'tri01'], writes=['stil%d' % par])
        if t < NT - 1:
            P.op('pe', lambda e: e.transpose(PSB[:, 4 + par, 0:128], qkT[:, 4 + h, tl], identb[:, :]),
                 reads=[qk_k, 'identb'], writes=['ps%d' % (4 + par)])
            P.op('act', lambda e: e.activation(out=ktil[par][:, :], in_=PSB[:, 4 + par, 0:128], func=AF.Copy, scale=u12[:, 1, col:col + 1]),
                 reads=['ps%d' % (4 + par), 'u12'], writes=['ktil%d' % par])

    def stage2(i):
        t, h = stepsB[i]
        par = i % 2
        col = t * 4 + h
        tl = slice(t * 128, (t + 1) * 128)
        qk_q = 'qkT%d_%d' % (h, t // 4)
        P.op('pe', lambda e: e.matmul(PS[:, 2 + par, 0:129], stil[par][:, :], vaug[:, t, h, 0:129], start=True, stop=(t == 0)),
             reads=['stil%d' % par, 'vaug%d' % t, 'vones'], writes=['ps%d' % (2 + par)])
        if t > 0:
            P.op('pe', lambda e: e.matmul(PS[:, 2 + par, 0:129], qkT[:, h, tl], Cb[h][:, 0:129], start=False, stop=True),
                 reads=[qk_q, 'Cb%d' % h], writes=['ps%d' % (2 + par)])
        if t < NT - 1:
            P.op('pe', lambda e: e.matmul(PS[:, 6 + par, 0:129], ktil[par][:, :], vaug[:, t, h, 0:129], start=True, stop=True),
                 reads=['ktil%d' % par, 'vaug%d' % t, 'vones'], writes=['ps%d' % (6 + par)])
            if t == 0:
                P.op('dve', lambda e: e.tensor_copy(Cf[h][:, 0:129], PS[:, 6 + par, 0:129]), reads=['ps%d' % (6 + par)], writes=['Cf%d' % h])
            else:
                P.op('dve', lambda e: e.scalar_tensor_tensor(out=Cf[h][:, 0:129], in0=Cf[h][:, 0:129], scalar=u12[:, 3, col:col + 1],
                                                             in1=PS[:, 6 + par, 0:129], op0=ALU.mult, op1=ALU.add),
                     reads=['ps%d' % (6 + par), 'u12', 'Cf%d' % h], writes=['Cf%d' % h])
            P.op('pool', lambda e: e.tensor_copy(Cb[h][:, 0:129], Cf[h][:, 0:129]), reads=['Cf%d' % h], writes=['Cb%d' % h])
        P.op('dve', lambda e: e.tensor_copy(Asb[:, t, h, 0:129], PS[:, 2 + par, 0:129]), reads=['ps%d' % (2 + par)], writes=['Asb%d_%d' % (t, h)])
        P.op('act', lambda e: e.activation(out=junkA[par][:, :], in_=Asb[:, t, h, 0:128], func=AF.Square, accum_out=ssqA[:, col:col + 1]),
             reads=['Asb%d_%d' % (t, h)], writes=['ssqA%d' % col, 'junkA%d' % par])

    for i in range(len(stepsB) + 1):
        if i < len(stepsB):
            stage1(i)
        if i >= 1:
            stage2(i - 1)
    allB = ['Asb%d_%d' % (t_, h_) for t_ in range(NT) for h_ in range(4)]
    allS = ['ssqA%d' % c for c in range(64)]
    Bsb3 = Bsb.reshape([128, NT, 4])
    P.op('dve', lambda e: e.tensor_copy(Bsb3[:, :, :], Asb[:, :, :, 128]), reads=allB, writes=['Bsb'])
    P.op('dve', lambda e: e.scalar_tensor_tensor(out=sm[:, 0, :], in0=Bsb[:, :], scalar=-1.0, in1=Bsb[:, :], op0=ALU.mult, op1=ALU.max), reads=['Bsb'], writes=['sm0'])
    P.op('dve', lambda e: e.tensor_tensor(out=sm[:, 0, :], in0=sm[:, 0, :], in1=u12[:, 2, :], op=ALU.max), reads=['u12', 'sm0'], writes=['sm0'])
    P.op('dve', lambda e: e.reciprocal(sm[:, 1, :], sm[:, 0, :]), reads=['sm0'], writes=['sm1'])
    P.op('dve', lambda e: e.tensor_tensor(out=sm[:, 2, :], in0=sm[:, 1, :], in1=sm[:, 1, :], op=ALU.mult), reads=['sm1'], writes=['sm2'])
    P.op('dve', lambda e: e.tensor_tensor(out=sm[:, 2, :], in0=sm[:, 2, :], in1=ssqA[:, :], op=ALU.mult), reads=['sm2'] + allS, writes=['sm2'])
    P.op('act', lambda e: e.activation(out=sm[:, 2, :], in_=sm[:, 2, :], func=AF.Sqrt, scale=1.0 / 128.0, bias=EPS), reads=['sm2'], writes=['sm2'])
    P.op('dve', lambda e: e.reciprocal(sm[:, 3, :], sm[:, 2, :]), reads=['sm2'], writes=['sm3'])
    P.op('dve', lambda e: e.tensor_tensor(out=sm[:, 3, :], in0=sm[:, 3, :], in1=sm[:, 1, :], op=ALU.mult), reads=['sm3', 'sm1'], writes=['sm3'])
    cnt = 0
    for g in range(4):
        for h in range(4):
            b = cnt % 2
            cnt += 1
            for j in range(4):
                t = 4 * g + j
                col = t * 4 + h
                sl = (cnt * 4 + j) % 4
                if j % 2 == 0:
                    P.op('act', lambda e, sl=sl, t=t, h=h, col=col: e.activation(out=hnb[sl][:, :], in_=Asb[:, t, h, 0:128], func=AF.Copy, scale=sm[:, 3, col:col + 1]),
                         reads=['Asb%d_%d' % (t, h), 'sm3'], writes=['hnb%d' % sl])
                else:
                    P.op('pool', lambda e, sl=sl, t=t, h=h, col=col: e.tensor_scalar(out=hnb[sl][:, :], in0=Asb[:, t, h, 0:128], scalar1=sm[:, 3, col:col + 1], scalar2=1.0,
                                                                                     op0=ALU.mult, op1=ALU.mult),
                         reads=['Asb%d_%d' % (t, h), 'sm3'], writes=['hnb%d' % sl])
                P.op('pe', lambda e, sl=sl, b=b, j=j: e.transpose(PSB[:, b, j * 128:(j + 1) * 128], hnb[sl][:, :], identb[:, :]),
                     reads=['hnb%d' % sl, 'identb'], writes=['ps%d' % b])
            P.op('dve', lambda e, b=b, g=g, h=h: e.tensor_tensor(out=catT[:, h, g * 512:(g + 1) * 512], in0=PSB[:, b, 0:512], in1=gT[:, h, g * 512:(g + 1) * 512], op=ALU.mult),
                 reads=['ps%d' % b, 'gT%d_%d' % (h, g)], writes=['catT%d_%d' % (h, g)])
    if 'catA' in dumps:
        add_dump('catA', catT[:, 0:4, :], [128, 4, S], BF16, ['catT%d_%d' % (a, b) for a in range(4) for b in range(4)])
    if 'Asb' in dumps:
        add_dump('Asb', Asb[:, :, :, :], [128, NT, 4, 128], BF16, ['Asb%d_%d' % (a, b) for a in range(NT) for b in range(4)])
    if 'sm' in dumps:
        add_dump('sm', sm[:, :, :], [128, 4, 64], F32, ['sm0', 'sm1', 'sm2', 'sm3'])
    A.release(mB)
    if upto <= 3:
        P.emit()
        return nc, dump_d

    P.barrier()
    mC = A.mark()
    PS8 = PS.reshape([128, 8, 8, 64])
    PSg = PS.reshape([128, 8, 64, 8])
    A.off = (A.off + 63) // 64 * 64
    Qaug_off = A.off
    Qaug = A.alloc("Qaug", [128, 8, S], BF16)
    Kaug = A.alloc("Kaug", [128, 8, S], BF16)
    Kaug4 = Kaug.reshape([128, 8, 8, 256])
    vb = A.alloc("vb", [128, NT, 8, 72], BF16)
    vb_end = A.off
    szb = A.alloc("szb", [128, 4, S], BF16)
    A.off = (A.off + 63) // 64 * 64
    qst_off = A.off
    qst = [A.alloc("qst%d" % i, [128, S], BF16) for i in range(2)]
    sel = A.alloc("sel", [128, 8, 64], BF16)
    kmf = A.alloc("kmf", [128, 64], F32)
    kmb = A.alloc("kmb", [128, 8, 8], BF16)
    gsb = A.alloc("gsb", [128, 8, 8, 8], F32)
    t8 = A.alloc("t8", [128, 8, 8, 8], F32)
    mq = A.alloc("mq", [128, 8, 8, 8], BF16)
    mT = A.alloc("mT", [128, 1024], BF16)
    kmb2 = kmb.reshape([128, 64])

    P.dma('sp', Kaug[64:75, :, :], kaug_d[:, :, :], writes=['Kconst'], group='cconst')
    P.dma('sp', Qaug[72:75, :, :], qaug_d[:, :, :], writes=['Qconst'], group='cconst')
    P.dma('sp', sel[64:72, :, :], sel_d[:, :, :], writes=['sel'], group='cconst')
    P.op('pool', lambda e: e.memset(Qaug[64:72, :, 0:1024], 0.0), writes=['Qmzero'])
    P.op('pool', lambda e: e.memset(vb[:, :, :, 64:72], 0.0), writes=['vbones'])
    for h in range(8):
        P.op('pool', lambda e, h=h: e.memset(vb[:, :, h, 64 + h:65 + h], 1.0), reads=['vbones'], writes=['vbones'])
    P.op('pool', lambda e: e.memset(gsb[:, :, :, :], NEG), writes=['gsbinit'])
    sti = 0
    for which, c0, dst, nm, scl in (('q', C_QB, Qaug, 'Qd', 0.125), ('k', C_KB, Kaug, 'Kd', None)):
        w, wk = preC[0 if which == 'q' else 1]
        for cl in range(4):
            st = qst[sti % 2]
            stk = 'qst%d' % (sti % 2)
            sti += 1
            for g in range(4):
                b = next_bank(0, 4)
                proj_fm(w, wk, cl, g, b)
                evac_copy(st[:, g * 512:(g + 1) * 512], PS[:, b, :], ['ps%d' % b], [stk + '_%d' % g], scale=scl)
            sk = [stk + '_%d' % g for g in range(4)]
            P.dma('sp', dst[0:64, 2 * cl, :], st[0:64, :], reads=sk, writes=['%s%d' % (nm, 2 * cl)], group=nm)
            P.dma('sp', dst[0:64, 2 * cl + 1, :], st[64:128, :], reads=sk, writes=['%s%d' % (nm, 2 * cl + 1)], group=nm)
    for h in range(8):
        P.op('dve', lambda e, h=h: e.tensor_reduce(out=kmf[0:64, h * 8:(h + 1) * 8], in_=Kaug4[0:64, h, :, :], op=ALU.add, axis=mybir.AxisListType.X),
             reads=['Kd%d' % h], writes=['kmf%d' % h])
    P.op('dve', lambda e: e.tensor_scalar(out=kmb2[0:64, :], in0=kmf[0:64, :], scalar1=1.0 / 256.0, scalar2=None, op0=ALU.mult),
         reads=['kmf%d' % h for h in range(8)], writes=['kmb'])
    w, wk = load_w(C_VB, 512)
    for t in range(NT):
        b = next_bank(0, 4)
        proj_tm(w, wk, t, 512, PS[:, b, :], 'ps%d' % b)
        evac_copy(vb[:, t, :, 0:64], PS8[:, b, :, :], ['ps%d' % b], ['vb%d' % t], force_act=True)
    def gate_tile(qt):
        j = qt // 2
        i2 = qt % 2
        gb = 4 + i2
        for h in range(8):
            P.op('pe', lambda e, h=h: e.matmul(PS[:, gb, h * 8:(h + 1) * 8], Qaug[0:64, h, qt * 128:(qt + 1) * 128], kmb[0:64, h, :], start=True, stop=True),
                 reads=['Qd%d' % h, 'kmb'], writes=['ps%d' % gb])
        P.op('dve', lambda e: e.tensor_copy(gsb[:, qt - 8, :, 0:j], PSg[:, gb, 0:8, 0:j]), reads=['ps%d' % gb, 'gsbinit'], writes=['gsb%d' % qt])
        for h in range(8):
            P.op('dve', lambda e, h=h: e.max(t8[:, qt - 8, h, :], gsb[:, qt - 8, h, :]), reads=['gsb%d' % qt], writes=['t8_%d_%d' % (qt, h)])
            P.op('dve', lambda e, h=h: e.tensor_scalar(out=mq[:, qt - 8, h, :], in0=gsb[:, qt - 8, h, :], scalar1=t8[:, qt - 8, h, 2:3], scalar2=1.0,
                                                       op0=ALU.is_ge, op1=ALU.subtract),
                 reads=['gsb%d' % qt, 't8_%d_%d' % (qt, h)], writes=['mq%d' % qt])
        P.op('dve', lambda e: e.memset(mq[:, qt - 8, :, j:j + 1], 0.0), reads=['mq%d' % qt], writes=['mq%d' % qt])

    w, wk = load_w(C_ZB, 512)
    zi_ = 0
    for cl in range(4):
        for g in range(4):
            if zi_ % 2 == 0:
                gate_tile(8 + zi_ // 2)
            zi_ += 1
            b = next_bank(0, 4)
            proj_fm(w, wk, cl, g, b)
            P.op('act', lambda e, b=b, cl=cl, g=g: e.activation(out=szb[:, cl, g * 512:(g + 1) * 512], in_=PS[:, b, :], func=AF.Silu),
                 reads=['ps%d' % b], writes=['szb%d_%d' % (cl, g)])
    mqf = mq.reshape([128, 8, 64])
    qm_ops = []

    def mask_finish():
        for qt in range(8, 16):
            P.op('pe', lambda e, qt=qt: e.transpose(PSB[0:64, 7, (qt - 8) * 128:(qt - 7) * 128], mqf[:, qt - 8, :], identb[:, :]),
                 reads=['mq%d' % qt, 'identb'], writes=['ps7'])
        P.op('dve', lambda e: e.tensor_copy(mT[0:64, :], PSB[0:64, 7, 0:1024]), reads=['ps7'], writes=['mT'])
        for h in range(8):
            qm_ops.append(P.dma('sp', Qaug[64:72, h, 1024:2048], mT[8 * h:8 * h + 8, :], reads=['mT'], writes=['Qm%d' % h], group='Qm'))

    wout_v = wout_d.rearrange("(kc p) c -> p kc c", p=128)
    for n in range(2):
        P.dma('pool', wbufs[n][:, :, :], wout_v[:, :, n * 512:(n + 1) * 512], writes=['wbuf%d' % n])
    for n in range(2):
        for kc in range(8):
            P.op('pool', lambda e, n=n, kc=kc: e.tensor_tensor(out=wbufs[n][:, kc, :], in0=wbufs[n][:, kc, :], in1=gate_bc[:, n * 512:(n + 1) * 512], op=ALU.mult),
                 reads=['wbuf%d' % n, 'gate_bc'], writes=['wbuf%d' % n])
    cur = A.off
    A.off = hT_off
    pt = [A.alloc("pt%d" % i, [128, 2, 512], BF16) for i in range(3)]
    onum2 = [A.alloc("onum%d" % i, [128, 8, 512], BF16) for i in range(2)]
    dens = A.alloc("dens", [128, 512], F32)
    rdf = A.alloc("rdf", [128, 512], F32)
    ost = [A.alloc("ost%d" % i, [128, 512], BF16) for i in range(4)]
    assert A.off <= hT_off + 8 * S * 2
    assert cur - qst_off >= 16384, (cur, qst_off)
    A.off = qst_off
    bc = A.alloc("bc", [128, 8, 512], F32)
    A.off = cur
    stepsC = []
    for qc in range(4):
        for h in range(8):
            lst = [('pair', pr) for pr in range(2 * qc)] + [('diag', r) for r in range(4)]
            for n_, (kind, idx) in enumerate(lst):
                stepsC.append((qc, h, kind, idx, n_ == 0, n_ == len(lst) - 1))
    osti_ = [0]
    defer = []

    def tick(flush=False):
        for d in defer:
            d[0] -= 1
        while defer and (flush or defer[0][0] <= 0):
            defer.pop(0)[1]()

    def szb_mult(c, g):
        ck = 'catT%d_%d' % (4 + c, g)
        P.op('dve', lambda e: e.tensor_tensor(out=catT[:, 4 + c, g * 512:(g + 1) * 512], in0=catT[:, 4 + c, g * 512:(g + 1) * 512],
                                              in1=szb[:, c, g * 512:(g + 1) * 512], op=ALU.mult),
             reads=[ck + 'a', ck + 'b', 'szb%d_%d' % (c, g)], writes=[ck])

    def att_front(i):
        qc, h, kind, idx, isfirst, islast = stepsC[i]
        st_ = i % 3
        qs = slice(qc * 512, (qc + 1) * 512)
        rk = ['Qd%d' % h, 'Qconst', 'Qmzero', 'Kd%d' % h, 'Kconst'] + (['Qm%d' % h] if qc >= 2 else [])
        if kind == 'pair':
            for i2 in range(2):
                kt = 2 * idx + i2
                P.op('pe', lambda e, i2=i2, kt=kt: e.matmul(PS[:, 2 * st_ + i2, :], Kaug[0:75, h, kt * 128:(kt + 1) * 128], Qaug[0:75, h, qs], start=True, stop=True),
                     reads=rk, writes=['ps%d' % (2 * st_ + i2)])
            for i2 in range(2):
                P.op('act', lambda e, i2=i2: e.activation(out=pt[st_][:, i2, :], in_=PS[:, 2 * st_ + i2, :], func=AF.Exp),
                     reads=['ps%d' % (2 * st_ + i2)], writes=['pt%d_%d' % (st_, i2)])
        else:
            r = idx
            kt = 4 * qc + r
            c0 = 128 * r
            bk = 2 * st_
            P.op('pe', lambda e: e.matmul(PS[:, bk, c0:512], Kaug[0:75, h, kt * 128:(kt + 1) * 128], Qaug[0:75, h, qc * 512 + c0:(qc + 1) * 512],
                                          start=True, stop=False, skip_group_check=True),
                 reads=rk, writes=['ps%d' % bk])
            P.op('pe', lambda e: e.matmul(PS[:, bk, c0:c0 + 128], identb[:, :], cmask[:, :], start=False, stop=True, skip_group_check=True),
                 reads=['identb', 'cmask'], writes=['ps%d' % bk])
            P.op('act', lambda e: e.activation(out=pt[st_][:, 0, c0:512], in_=PS[:, bk, c0:512], func=AF.Exp),
                 reads=['ps%d' % bk], writes=['pt%d_0' % st_])

    def att_back(i):
        qc, h, kind, idx, isfirst, islast = stepsC[i]
        st_ = i % 3
        qs = slice(qc * 512, (qc + 1) * 512)
        ob = 6 + h % 2
        okey = 'ps%d' % ob
        if kind == 'pair':
            for i2 in range(2):
                kt = 2 * idx + i2
                P.op('pe', lambda e, i2=i2, kt=kt: e.matmul(PS[0:72, ob, :], vb[:, kt, h, 0:72], pt[st_][:, i2, :],
                                                            start=(isfirst and i2 == 0), stop=False, skip_group_check=True),
                     reads=['pt%d_%d' % (st_, i2), 'vb%d' % kt, 'vbones'], writes=[okey])
        else:
            r = idx
            kt = 4 * qc + r
            c0 = 128 * r
            P.op('pe', lambda e: e.matmul(PS[0:72, ob, c0:512], vb[:, kt, h, 0:72], pt[st_][:, 0, c0:512],
                                          start=isfirst, stop=(r == 3), skip_group_check=True),
                 reads=['pt%d_0' % st_, 'vb%d' % kt, 'vbones'], writes=[okey])
        if not islast:
            return
        onum = onum2[qc % 2]
        onk = 'onum%d_' % (qc % 2)
        P.op('dve', lambda e: e.tensor_copy(onum[0:64, h, :], PS[0:64, ob, :]), reads=[okey], writes=[onk + '%d' % h])
        if h == 0:
            P.op('dve', lambda e: e.tensor_copy(dens[64:72, :], PS[64:72, ob, :]), reads=[okey], writes=['dens'])
        else:
            P.op('dve', lambda e: e.tensor_tensor(out=dens[64:72, :], in0=dens[64:72, :], in1=PS[64:72, ob, :], op=ALU.add), reads=[okey, 'dens'], writes=['dens'])
        tick()
        if h % 4 != 3:
            return
        gi = h // 4
        hi_ = 64 + 4 * (gi + 1)
        P.op('dve', lambda e: e.reciprocal(rdf[64:hi_, :], dens[64:hi_, :]), reads=['dens'], writes=['rdf'])
        P.dma('sp', scr_d[4 * gi:4 * gi + 4, :], rdf[64 + 4 * gi:68 + 4 * gi, :], reads=['rdf'], writes=['scr%d' % gi], extra=list(qm_ops))
        for hh in range(4 * gi, 4 * gi + 4):
            P.dma('sp', bc[0:64, hh:hh + 1, :], scr_d[hh:hh + 1, :].partition_broadcast(64), reads=['scr%d' % gi], writes=['bc%d' % hh], extra=list(qm_ops))

        def part2(qc=qc, gi=gi, qs=qs, onum=onum, onk=onk):
            for hh in range(4 * gi, 4 * gi + 4):
                ck = 'catT%d_%d' % (4 + hh // 2, qc)
                if hh % 2 == 0:
                    P.op('dve', lambda e, hh=hh: e.tensor_tensor(out=catT[0:64, 4 + hh // 2, qs], in0=onum[0:64, hh, :], in1=bc[0:64, hh, :], op=ALU.mult),
                         reads=['bc%d' % hh, onk + '%d' % hh], writes=[ck + 'a'])
                else:
                    o_ = ost[osti_[0] % 4]
                    ok_ = 'ost%d' % (osti_[0] % 4)
                    osti_[0] += 1
                    P.op('dve', lambda e, hh=hh, o_=o_: e.tensor_tensor(out=o_[0:64, :], in0=onum[0:64, hh, :], in1=bc[0:64, hh, :], op=ALU.mult),
                         reads=['bc%d' % hh, onk + '%d' % hh], writes=[ok_])
                    P.dma('sp', catT[64:128, 4 + hh // 2, qs], o_[0:64, :], reads=[ok_], writes=[ck + 'b'], group='catTb%d_%d' % (qc, gi))

        def part3(qc=qc, gi=gi):
            szb_mult(2 * gi, qc)
            szb_mult(2 * gi + 1, qc)

        defer.append([2, part2])
        defer.append([4, part3])

    for i in range(len(stepsC) + 1):
        if i < len(stepsC):
            att_front(i)
        if i >= 1:
            att_back(i - 1)
        if i == 10:
            mask_finish()
    assert len(qm_ops) == 8
    last_att_pe = max(i for i, o in enumerate(P.ops) if o['eng'] == 'pe' and not o['dma'])
    if 'catB' in dumps:
        add_dump('catB', catT[:, 4:8, :], [128, 4, S], BF16, ['catT%d_%d' % (a, b) for a in range(4, 8) for b in range(4)])
    A.release(mC)
    if upto <= 4:
        P.emit()
        return nc, dump_d

    P.fence([last_att_pe])
    A.off = Qaug_off
    gfin = A.alloc("gfin", [128, D], F32)
    xr = [A.alloc("xr%d" % i, [128, D], F32) for i in range(4)]
    pre = A.alloc("pre", [128, 8, D], F32)
    ot = [A.alloc("ot%d" % i, [128, D], F32) for i in range(6)]
    ssqo = A.alloc("ssqo", [128, NT], F32)
    rso = A.alloc("rso", [128, NT], F32)
    jk = [A.alloc("jk%d" % i, [128, D], BF16) for i in range(2)]
    assert A.off <= vb_end, (A.off, vb_end)
    ld(gfin[:, :], gfin_d[:, :], 'gfin', group='gfin')

    def fin_tile(tt):
        g4 = tt // 4
        ok_ = 'ot%d' % (tt % 6)
        P.op('dve', lambda e: e.scalar_tensor_tensor(out=ot[tt % 6][:, :], in0=pre[:, tt % 8, :], scalar=rso[:, tt:tt + 1], in1=gfin[:, :], op0=ALU.mult, op1=ALU.mult),
             reads=['pre%d_0' % (tt % 8), 'pre%d_1' % (tt % 8), 'rso%d' % g4, 'gfin'], writes=[ok_])
        P.dma('pool', out_d[tt * 128:(tt + 1) * 128, :], ot[tt % 6][:, :], reads=[ok_], writes=['out'], final=True)

    pend = []

    def x_reload(t):
        P.dma('act', xr[t % 4][:, :], x_d[t * 128:(t + 1) * 128, :], writes=['xr%d' % (t % 4)])

    for t in range(3):
        x_reload(t)
    for t in range(NT):
        xk = 'xr%d' % (t % 4)
        if t + 3 < NT:
            x_reload(t + 3)
        for n in range(2):
            b = next_bank(0, 4)
            for kc in range(8):
                P.op('pe', lambda e, b=b, kc=kc, t=t, n=n: e.matmul(PS[:, b, :], catT[:, kc, t * 128:(t + 1) * 128], wbufs[n][:, kc, :],
                                                                   start=(kc == 0), stop=(kc == 7)),
                     reads=['wbuf%d' % n, 'catT%d_%d' % (kc, t // 4)], writes=['ps%d' % b])
            P.op('dve', lambda e, b=b, t=t, n=n: e.tensor_tensor(out=pre[:, t % 8, n * 512:(n + 1) * 512], in0=PS[:, b, :], in1=xr[t % 4][:, n * 512:(n + 1) * 512], op=ALU.add),
                 reads=['ps%d' % b, xk], writes=['pre%d_%d' % (t % 8, n)])
        P.op('act', lambda e, t=t: e.activation(out=jk[t % 2][:, :], in_=pre[:, t % 8, :], func=AF.Square, accum_out=ssqo[:, t:t + 1]),
             reads=['pre%d_0' % (t % 8), 'pre%d_1' % (t % 8)], writes=['ssqo%d' % t, 'jk%d' % (t % 2)])
        if pend:
            fin_tile(pend.pop(0))
        if t == 2 or t == 5:
            tick()
            tick()
        if t == 8:
            tick(flush=True)
        if t % 4 == 3:
            g4 = t // 4
            P.op('act', lambda e, g4=g4: e.activation(out=rso[:, g4 * 4:(g4 + 1) * 4], in_=ssqo[:, g4 * 4:(g4 + 1) * 4], func=AF.Sqrt, scale=1.0 / D, bias=EPS),
                 reads=['ssqo%d' % tt for tt in range(g4 * 4, g4 * 4 + 4)], writes=['rso%d' % g4])
            P.op('dve', lambda e, g4=g4: e.reciprocal(rso[:, g4 * 4:(g4 + 1) * 4], rso[:, g4 * 4:(g4 + 1) * 4]), reads=['rso%d' % g4], writes=['rso%d' % g4])
            pend.extend(range(g4 * 4, g4 * 4 + 4))
    while pend:
        fin_tile(pend.pop(0))
    P.emit()
    return nc, dump_d


def _bf(a):
    return np.ascontiguousarray(np.asarray(a, dtype=np.float32)).astype(ml_dtypes.bfloat16)


def host_consts():
    c = {}
    eye = np.eye(128, dtype=np.float32)
    c["identb"] = _bf(eye)
    c["identf"] = eye
    up = (np.arange(128)[:, None] <= np.arange(128)[None, :]).astype(np.float32)
    c["U"] = up
    c["tri01"] = _bf(up)
    c["cmask"] = _bf(np.where(np.arange(128)[:, None] > np.arange(128)[None, :], -30000.0, 0.0))
    pos = np.arange(S)
    kaug = np.zeros((11, 8, S), np.float32)
    qaug = np.zeros((3, 8, S), np.float32)
    for h in range(8):
        slope = 2.0 ** (-(h + 1))
        for r in range(8):
            kaug[r, h] = np.where(pos // 256 == r, 32768.0, 0.0)
        kaug[8, h] = 1.0
        kaug[9, h] = slope * 128.0 * (pos // 128)
        kaug[10, h] = slope * (pos % 128)
        qaug[0, h] = -slope * pos
        qaug[1, h] = 1.0
        qaug[2, h] = 1.0
    c["kaugc"] = _bf(kaug)
    c["qaugc"] = _bf(qaug)
    sel = np.zeros((8, 8, 64), np.float32)
    for h in range(8):
        sel[h, h, :] = 1.0
    c["selc"] = _bf(sel)
    return c


def host_inputs(inp, b, consts=None):
    f = lambda a: np.ascontiguousarray(np.asarray(a, dtype=np.float32))
    m = dict(consts if consts is not None else host_consts())
    m["x"] = f(inp["x"][b])
    m["cT"] = f(np.asarray(inp["c"][b]).reshape(8, 128).T)
    m["w_ada"] = f(inp["w_ada"][0])
    bada = np.asarray(inp["b_ada"][0])
    m["bss"] = f(bada[:2048].reshape(16, 128).T)
    m["bgate"] = f(np.broadcast_to(bada[2048:][None, :], (128, D)))
    m["gnT"] = f(np.asarray(inp["g_norm"][0]).reshape(8, 128).T)
    m["w_in"] = f(inp["w_in"][0])
    m["convw"] = f(np.asarray(inp["conv_w"][0]).reshape(4, 8, 128).transpose(2, 1, 0).reshape(128, 32))
    m["convb"] = f(np.asarray(inp["conv_b"][0]).reshape(8, 128).T)
    bi = np.asarray(inp["b_igate"][0]); bfv = np.asarray(inp["b_fgate"][0])
    m["bif"] = f(np.broadcast_to(np.tile(np.concatenate([bi, bfv]), NT)[None, :], (128, 128)))
    m["gml"] = f(np.asarray(inp["g_mlstm_head"][0]).reshape(4, 128).T)
    m["w_out"] = f(inp["w_out"][0])
    m["gfin"] = f(np.broadcast_to(np.asarray(inp["g_final"])[None, :], (128, D)))
    return m


_CACHE = {}


def kernel(**inputs):
    consts = host_consts()
    in_maps = [host_inputs(inputs, b, consts) for b in range(8)]
    nc, _ = build_program()
    res = run_bass_kernel_spmd(nc, in_maps, core_ids=list(range(8)))
    out = np.stack([np.asarray(r["out"], dtype=np.float32) for r in res.results], axis=0)
    return out
```

```python
import numpy as np
import ml_dtypes
import concourse.bass as bass
import concourse.mybir as mybir
from concourse.bass_utils import run_bass_kernel_spmd

F32 = mybir.dt.float32
BF16 = mybir.dt.bfloat16
AF = mybir.ActivationFunctionType
ALU = mybir.AluOpType

_COMPUTE = ('pe', 'act', 'dve', 'pool')


class Prog:
    def __init__(self, nc):
        self.nc = nc
        self.ops = []
        self.last_w = {}
        self.readers = {}
        self.eng_sem = {}
        self.key_sem = {}
        self.final_keys = []
        self.bar = None

    def barrier(self, exclude=()):
        lasts = {}
        for i, o in enumerate(self.ops):
            key = ('k', o['semkey']) if o['dma'] else ('e', o['eng'])
            if o['dma'] and o['semkey'] in exclude:
                continue
            lasts[key] = i
        self.bar = (set(lasts.values()), set())

    def fence(self, dep_ops):
        self.bar = (set(dep_ops), set())

    def _add(self, eng, fn, reads, writes, is_dma, semkey=None, extra=()):
        i = len(self.ops)
        deps = set(extra)
        psr = [k for k in reads if isinstance(k, str) and k.startswith('ps')]
        if psr:
            reads = [k for k in reads if k not in psr]
            writes = list(writes) + psr
        if self.bar is not None and eng not in self.bar[1]:
            deps |= self.bar[0]
            self.bar[1].add(eng)
        for k in reads:
            if k in self.last_w:
                deps.add(self.last_w[k])
        for k in writes:
            if k in self.last_w:
                deps.add(self.last_w[k])
            for r in self.readers.get(k, ()):
                deps.add(r)
        deps.discard(i)
        self.ops.append(dict(eng=eng, fn=fn, deps=sorted(deps), dma=is_dma, semkey=semkey, sig=is_dma))
        for k in reads:
            self.readers.setdefault(k, []).append(i)
        for k in writes:
            self.last_w[k] = i
            self.readers[k] = []
        return i

    def op(self, eng, fn, reads=(), writes=()):
        return self._add(eng, fn, list(reads), list(writes), False)

    def dma(self, queue, out, in_, reads=(), writes=(), final=False, group=None, extra=()):
        writes = list(writes)
        semkey = writes[0] if group is None else ('G', group)
        if final:
            self.final_keys.append(semkey)
        return self._add(queue, lambda e: e.dma_start(out=out, in_=in_), list(reads), writes, True, semkey, extra=extra)

    def emit(self):
        nc = self.nc
        ops = self.ops
        for o in ops:
            for d in o['deps']:
                p = ops[d]
                if p['dma']:
                    continue
                if p['eng'] == 'pe' and o['eng'] == 'pe' and not o['dma']:
                    continue
                p['sig'] = True
        cnt = {}
        for o in ops:
            if o['dma']:
                k = ('k', o['semkey'])
                cnt[k] = cnt.get(k, 0) + 16
                o['tok'] = (k, cnt[k])
            elif o['sig']:
                k = ('e', o['eng'])
                cnt[k] = cnt.get(k, 0) + 1
                o['tok'] = (k, cnt[k])
            else:
                o['tok'] = None
        sems = {}
        for k in cnt:
            sems[k] = nc.alloc_semaphore("s_%s_%s" % (k[0], str(k[1]).replace(' ', '_')))
        self.sems = sems
        final_tok = {}
        for o in ops:
            if o['dma'] and o['semkey'] in self.final_keys:
                final_tok[o['tok'][0]] = o['tok'][1]
        seen = {e: {} for e in ('pe', 'act', 'dve', 'pool', 'sp')}
        per_eng = {e: [] for e in seen}
        for o in ops:
            e = o['eng']
            waits = []
            for d in o['deps']:
                p = ops[d]
                if (not p['dma']) and (not o['dma']) and p['eng'] == 'pe' and e == 'pe':
                    continue
                k, v = p['tok']
                if p['dma'] and isinstance(p['semkey'], tuple):
                    v = max(q['tok'][1] for q in ops[:ops.index(o)] if q['dma'] and q['semkey'] == p['semkey'])
                if seen[e].get(k, 0) >= v:
                    continue
                seen[e][k] = v
                waits.append((k, v))
            per_eng[e].append((o, waits))

        def run(engname, handle):
            for o, waits in per_eng[engname]:
                for k, v in waits:
                    handle.wait_ge(sems[k], v)
                ins = o['fn'](handle)
                if o['tok'] is not None:
                    k, v = o['tok']
                    ins.then_inc(sems[k], 16 if o['dma'] else 1)
            if engname == 'sp':
                for k, v in final_tok.items():
                    handle.wait_ge(sems[k], v)

        with nc.Block() as block:
            @block.tensor
            def _(t):
                run('pe', t)

            @block.scalar
            def _(s):
                run('act', s)

            @block.vector
            def _(v):
                run('dve', v)

            @block.gpsimd
            def _(g):
                run('pool', g)

            @block.sync
            def _(sy):
                run('sp', sy)
        self.ops = []


S = 2048
D = 1024
NT = 16
PROJ = 4616
EPS = 1e-6
C_QM, C_KM, C_VM, C_OM, C_IF, C_ZM, C_QB, C_KB, C_VB, C_ZB = 0, 512, 1024, 1536, 2048, 2056, 2568, 3080, 3592, 4104
LN_S = float(np.log(128.0 ** -0.5))
NEG = -1.0e30


class Arena:
    def __init__(self, nc, limit=16384 + 207 * 1024):
        self.nc = nc
        self.off = 16384
        self.limit = limit
        self.n = 0

    def alloc(self, name, shape, dtype):
        esz = 4 if dtype == F32 else 2
        per_part = int(np.prod(shape[1:])) * esz
        self.off = (self.off + 63) // 64 * 64
        t = self.nc.alloc_sbuf_tensor_at("%s_%d" % (name, self.n), list(shape), dtype, offset=self.off)
        self.n += 1
        self.off += per_part
        assert self.off <= self.limit, (name, self.off)
        return t

    def mark(self):
        return self.off

    def release(self, m):
        self.off = m


def build_program(upto=99, dumps=()):
    nc = bass.Bass("TRN2", target_bir_lowering=False)
    P = Prog(nc)
    A = Arena(nc)

    def din(name, shape, dt=F32):
        return nc.dram_tensor(name, list(shape), dt, kind="ExternalInput")

    x_d = din("x", [S, D])
    cT_d = din("cT", [128, 8])
    wada_d = din("w_ada", [D, 3 * D])
    bss_d = din("bss", [128, 16])
    bgate_d = din("bgate", [128, D])
    gnT_d = din("gnT", [128, 8])
    win_d = din("w_in", [D, PROJ])
    convw_d = din("convw", [128, 32])
    convb_d = din("convb", [128, 8])
    bif_d = din("bif", [128, 128])
    gml_d = din("gml", [128, 4])
    wout_d = din("w_out", [D, D])
    gfin_d = din("gfin", [128, D])
    identb_d = din("identb", [128, 128], BF16)
    identf_d = din("identf", [128, 128])
    U_d = din("U", [128, 128])
    tri_d = din("tri01", [128, 128], BF16)
    cmask_d = din("cmask", [128, 128], BF16)
    kaug_d = din("kaugc", [11, 8, S], BF16)
    qaug_d = din("qaugc", [3, 8, S], BF16)
    sel_d = din("selc", [8, 8, 64], BF16)
    out_d = nc.dram_tensor("out", [S, D], F32, kind="ExternalOutput")
    scr_d = nc.dram_tensor("scr", [8, 512], F32, kind="Internal")
    dump_d = {}

    PS = nc.alloc_psum_tensor("ps", [128, 8, 512], F32)
    PSB = PS.bitcast(BF16)

    identb = A.alloc("identb", [128, 128], BF16)
    identf = A.alloc("identf", [128, 128], F32)
    U = A.alloc("U", [128, 128], F32)
    onesf = A.alloc("onesf", [128, 128], F32)
    tri01 = A.alloc("tri01", [128, 128], BF16)
    cmask = A.alloc("cmask", [128, 128], BF16)
    gs = A.alloc("gs", [128, 8], F32)
    shiftT = A.alloc("shiftT", [128, 8], F32)
    gate_bc = A.alloc("gate_bc", [128, D], F32)
    convw = A.alloc("convw", [128, 32], F32)
    convb = A.alloc("convb", [128, 8], F32)
    bif = A.alloc("bif", [128, 128], F32)
    gml = A.alloc("gml", [128, 4], F32)
    A.off = (A.off + 63) // 64 * 64
    catT_off = A.off
    catT = A.alloc("catT", [128, 8, S], BF16)
    wbufs = [A.alloc("wbuf%d" % i, [128, 8, 512], BF16) for i in range(2)]
    screp = A.alloc("screp", [128, 8, 128], BF16)
    A.off = (A.off + 63) // 64 * 64
    hT_off = A.off
    hT = A.alloc("hT", [128, 8, S], BF16)
    base_mark = A.mark()

    def ld(dst, src, key, q='sp', group='consts'):
        return P.dma(q, dst, src, writes=[key], group=group)

    win_v = win_d.rearrange("(kc p) c -> p kc c", p=128)
    pre_w = [(wbufs[grp], 'wbuf%d' % grp) for grp in range(2)]

    ld(identb[:, :], identb_d[:, :], 'identb')
    ld(identf[:, :], identf_d[:, :], 'identf')
    ld(U[:, :], U_d[:, :], 'U')
    ld(tri01[:, :], tri_d[:, :], 'tri01')
    ld(cmask[:, :], cmask_d[:, :], 'cmask')
    ld(convw[:, :], convw_d[:, :], 'convw')
    ld(convb[:, :], convb_d[:, :], 'convb')
    ld(bif[:, :], bif_d[:, :], 'bif')
    ld(gml[:, :], gml_d[:, :], 'gml')
    P.op('pool', lambda e: e.memset(onesf[:, :], 1.0), writes=['onesf'])

    def add_dump(name, ap, shape, dt, key):
        d = nc.dram_tensor("dbg_" + name, list(shape), dt, kind="ExternalOutput")
        dump_d[name] = d
        P.dma('sp', d[tuple(slice(None) for _ in shape)], ap, reads=(key if isinstance(key, list) else [key]), writes=['dbg_' + name], final=True)

    m = A.mark()
    xall = A.alloc("xall", [128, NT, D], F32)
    _sv = A.off
    A.off = catT_off
    xn = A.alloc("xn", [128, NT, D], BF16)
    A.off = _sv
    junks = [A.alloc("junk%d" % i, [128, D], BF16) for i in range(2)]
    wst = [A.alloc("wst%d" % i, [128, 2 * D], BF16) for i in range(2)]
    wsf = [A.alloc("wsf%d" % i, [128, 2 * D], F32) for i in range(4)]
    sc = A.alloc("sc", [128, 8], BF16)
    cT = A.alloc("cT", [128, 8], F32)
    ssq = A.alloc("ssq", [128, NT], F32)
    rstd = A.alloc("rstd", [128, NT], F32)
    bss = A.alloc("bss", [128, 16], F32)
    gnT = A.alloc("gnT", [128, 8], F32)
    scl1 = A.alloc("scl1", [128, 8], F32)

    ld(cT[:, :], cT_d[:, :], 'cT')
    ld(bss[:, :], bss_d[:, :], 'bss')
    ld(gnT[:, :], gnT_d[:, :], 'gnT')
    ld(gate_bc[:, :], bgate_d[:, :], 'gate_bc')
    P.op('act', lambda e: e.activation(out=sc[:, :], in_=cT[:, :], func=AF.Silu), reads=['cT'], writes=['sc'])
    for kc in range(8):
        P.op('dve', lambda e, kc=kc: e.tensor_scalar(out=screp[:, kc, :], in0=onesf[:, :], scalar1=sc[:, kc:kc + 1], scalar2=None, op0=ALU.mult),
             reads=['onesf', 'sc'], writes=['screp%d' % kc])
    tcnt = [0]

    def x_group(g, modulate=False):
        for t in range(4 * g, 4 * g + 4):
            P.op('act', lambda e, t=t: e.activation(out=junks[t % 2][:, :], in_=xall[:, t, :], func=AF.Square, accum_out=ssq[:, t:t + 1]),
                 reads=['xall%d' % t], writes=['ssq%d' % t, 'junk%d' % (t % 2)])
        P.op('act', lambda e: e.activation(out=rstd[:, 4 * g:4 * g + 4], in_=ssq[:, 4 * g:4 * g + 4], func=AF.Sqrt, scale=1.0 / D, bias=EPS),
             reads=['ssq%d' % t for t in range(4 * g, 4 * g + 4)], writes=['rstd%d' % g])
        P.op('dve', lambda e: e.reciprocal(rstd[:, 4 * g:4 * g + 4], rstd[:, 4 * g:4 * g + 4]), reads=['rstd%d' % g], writes=['rstd%d' % g])
        for t in range(4 * g, 4 * g + 4):
            P.op('dve', lambda e, t=t: e.tensor_scalar(out=xn[:, t, :], in0=xall[:, t, :], scalar1=rstd[:, t:t + 1], scalar2=None, op0=ALU.mult),
                 reads=['xall%d' % t, 'rstd%d' % g], writes=['xn%d' % t])
        for dc in range(8):
            b = 3 + (tcnt[0] % 4)
            for j in range(4):
                t = 4 * g + j
                P.op('pe', lambda e, b=b, j=j, t=t, dc=dc: e.transpose(PSB[:, b, j * 128:(j + 1) * 128], xn[:, t, dc * 128:(dc + 1) * 128], identb[:, :]),
                     reads=['xn%d' % t, 'identb'], writes=['ps%d' % b])
            if modulate:
                if tcnt[0] % 2 == 0:
                    P.op('act', lambda e, b=b, dc=dc: e.activation(out=hT[:, dc, g * 512:(g + 1) * 512], in_=PSB[:, b, 0:512], func=AF.Identity,
                                                                   scale=gs[:, dc:dc + 1], bias=shiftT[:, dc:dc + 1]),
                         reads=['ps%d' % b, 'gs', 'shiftT'], writes=['hT%d_%d' % (dc, g)])
                else:
                    P.op('dve', lambda e, b=b, dc=dc: e.tensor_scalar(out=hT[:, dc, g * 512:(g + 1) * 512], in0=PSB[:, b, 0:512],
                                                                       scalar1=gs[:, dc:dc + 1], scalar2=shiftT[:, dc:dc + 1], op0=ALU.mult, op1=ALU.add),
                         reads=['ps%d' % b, 'gs', 'shiftT'], writes=['hT%d_%d' % (dc, g)])
            elif tcnt[0] % 2 == 0:
                P.op('act', lambda e, b=b, dc=dc: e.activation(out=hT[:, dc, g * 512:(g + 1) * 512], in_=PSB[:, b, 0:512], func=AF.Copy),
                     reads=['ps%d' % b], writes=['hT%d_%d' % (dc, g)])
            else:
                P.op('dve', lambda e, b=b, dc=dc: e.tensor_copy(hT[:, dc, g * 512:(g + 1) * 512], PSB[:, b, 0:512]),
                     reads=['ps%d' % b], writes=['hT%d_%d' % (dc, g)])
            tcnt[0] += 1

    sfi = 0
    for kc in range(8):
        w = wst[kc % 2]
        wk = 'wst%d' % (kc % 2)
        wf = wsf[sfi % 4]
        wfk = 'wsf%d' % (sfi % 4)
        sfi += 1
        P.dma('pool', wf[:, :], wada_d[kc * 128:(kc + 1) * 128, 0:2048], writes=[wfk])
        P.op('dve', lambda e, w=w, wf=wf: e.tensor_copy(w[:, :], wf[:, :]), reads=[wfk], writes=[wk])
        for t in (2 * kc, 2 * kc + 1):
            last_x_ld = ld(xall[:, t, :], x_d[t * 128:(t + 1) * 128, :], 'xall%d' % t, group='xall%d' % (t // 4))
        for j in range(16):
            P.op('pe', lambda e, w=w, j=j, kc=kc: e.matmul(PS[:, 0, j:j + 1], w[:, j * 128:(j + 1) * 128], sc[:, kc:kc + 1],
                                                            start=(kc == 0 and j == 0), stop=(kc == 7), skip_group_check=True),
                 reads=[wk, 'sc'], writes=['ps0'])
        if kc == 7:
            for grp in range(2):
                P.dma('pool', wbufs[grp][:, :, 0:512], win_v[:, :, C_QM + grp * 512:C_QM + (grp + 1) * 512], writes=['wbuf%d' % grp],
                      extra=[last_x_ld])
        if kc % 2 == 1 and kc >= 3:
            x_group((kc - 3) // 2)
    P.op('dve', lambda e: e.tensor_tensor(out=shiftT[:, :], in0=PS[:, 0, 0:8], in1=bss[:, 0:8], op=ALU.add), reads=['ps0', 'bss'], writes=['shiftT'])
    P.op('dve', lambda e: e.scalar_tensor_tensor(out=scl1[:, :], in0=PS[:, 0, 8:16], scalar=1.0, in1=bss[:, 8:16], op0=ALU.add, op1=ALU.add),
         reads=['ps0', 'bss'], writes=['scl1'])
    P.op('dve', lambda e: e.tensor_tensor(out=gs[:, :], in0=scl1[:, :], in1=gnT[:, :], op=ALU.mult), reads=['scl1', 'gnT'], writes=['gs'])
    x_group(3, modulate=True)
    for dc in range(8):
        eng = 'dve' if dc % 2 == 0 else 'pool'
        P.op(eng, lambda e, dc=dc: e.tensor_scalar(out=hT[:, dc, 0:1536], in0=hT[:, dc, 0:1536], scalar1=gs[:, dc:dc + 1], scalar2=shiftT[:, dc:dc + 1],
                                                   op0=ALU.mult, op1=ALU.add),
             reads=['hT%d_%d' % (dc, g) for g in range(3)] + ['gs', 'shiftT'], writes=['hT%d_%d' % (dc, g) for g in range(3)])
    if 'hT' in dumps:
        add_dump('hT', hT[:, :, :], [128, 8, S], BF16, ['hT%d_%d' % (a, b) for a in range(8) for b in range(4)])
    if 'gate_bc' in dumps:
        add_dump('gate_bc', gate_bc[:, :], [128, D], F32, 'gate_bc')
    A.release(m)
    if upto <= 1:
        P.emit()
        return nc, dump_d

    P.barrier()
    PS4 = PS.reshape([128, 8, 4, 128])
    wslot = [0]

    def load_w(c0, ncols):
        sl = wslot[0] % 2
        wslot[0] += 1
        P.dma('pool', wbufs[sl][:, :, 0:ncols], win_v[:, :, c0:c0 + ncols], writes=['wbuf%d' % sl])
        return wbufs[sl], 'wbuf%d' % sl

    bank_rr = [0]

    def next_bank(lo=0, n=4):
        b = lo + bank_rr[0] % n
        bank_rr[0] += 1
        return b

    evac_rr = [0]

    def evac_copy(out, in_, reads, writes, scale=None, force_act=False):
        evac_rr[0] += 1
        if force_act or evac_rr[0] % 2 == 0:
            if scale is None:
                P.op('act', lambda e: e.activation(out=out, in_=in_, func=AF.Copy), reads=reads, writes=writes)
            else:
                P.op('act', lambda e: e.activation(out=out, in_=in_, func=AF.Copy, scale=scale), reads=reads, writes=writes)
        else:
            if scale is None:
                P.op('dve', lambda e: e.tensor_copy(out, in_), reads=reads, writes=writes)
            else:
                P.op('dve', lambda e: e.tensor_scalar(out=out, in0=in_, scalar1=scale, scalar2=None, op0=ALU.mult), reads=reads, writes=writes)

    def hkeys(g):
        return ['hT%d_%d' % (kc, g) for kc in range(8)]

    def proj_fm(w, wk, cl, g, b):
        for kc in range(8):
            P.op('pe', lambda e, kc=kc: e.matmul(PS[:, b, :], w[:, kc, cl * 128:(cl + 1) * 128], hT[:, kc, g * 512:(g + 1) * 512],
                                                  start=(kc == 0), stop=(kc == 7)),
                 reads=[wk] + hkeys(g), writes=['ps%d' % b])

    def proj_tm(w, wk, t, ncols, out_ap, outkey):
        for kc in range(8):
            P.op('pe', lambda e, kc=kc: e.matmul(out_ap, hT[:, kc, t * 128:(t + 1) * 128], w[:, kc, 0:ncols],
                                                  start=(kc == 0), stop=(kc == 7)),
                 reads=[wk] + hkeys(t // 4), writes=[outkey])

    mB = A.mark()
    qkT = A.alloc("qkT", [128, 8, S], BF16)
    vaug = A.alloc("vaug", [128, NT, 4, 130], BF16)
    A.off = (A.off + 63) // 64 * 64
    gT_off = A.off
    gT = A.alloc("gT", [128, 4, S], BF16)
    _sv2 = A.off
    A.off = gT_off
    wg = A.alloc("wg", [128, 8, D], BF16)
    A.off = _sv2
    u12 = A.alloc("u12", [128, 4, 64], F32)
    mB2 = A.mark()
    raw = A.alloc("raw", [128, 8, S + 4], BF16)
    cacc = [A.alloc("cacc%d" % i, [128, 512], F32) for i in range(4)]
    wif = A.alloc("wif", [128, 8, 8], BF16)
    gtmp = [A.alloc("gtmp%d" % i, [128, 512], F32) for i in range(2)]
    szt = [A.alloc("szt%d" % i, [128, 512], BF16) for i in range(2)]
    fi = A.alloc("fi", [128, 128], F32)
    nl = A.alloc("nl", [128, 64], F32)
    linc = A.alloc("linc", [128, 68], F32)
    NF = A.alloc("NF", [128, 64], F32)
    gg = A.alloc("gg", [128, 64], F32)
    tmax = A.alloc("tmax", [128, 1], F32)
    trow = A.alloc("trow", [1, 64], F32)
    Rrow = A.alloc("Rrow", [1, 68], F32)
    Rb = A.alloc("Rb", [128, 68], F32)
    fi3 = fi.reshape([128, 16, 8])
    nl3 = nl.reshape([128, 16, 4])
    NF3 = NF.reshape([128, 16, 4])
    gg3 = gg.reshape([128, 16, 4])

    P.dma('pool', wif[:, :, :], win_v[:, :, C_IF:C_IF + 8], writes=['wif'])
    for k2 in range(8):
        P.dma('pool', wg[:, k2, :], wada_d[k2 * 128:(k2 + 1) * 128, 2048:3072], writes=['wg%d' % k2], group='wg')
    P.op('pool', lambda e: e.memset(raw[:, :, 0:3], 0.0), writes=['rawpad'])
    P.op('pool', lambda e: e.memset(vaug[:, :, :, 128:129], 1.0), writes=['vones'])
    def conv_tile(ct):
        for gp in range(2):
            gs_ = (2 * gp, 2 * gp + 1)
            for g in gs_:
                acc = cacc[g]
                P.op('dve', lambda e, g=g, acc=acc: e.tensor_scalar(out=acc[:, :], in0=raw[:, ct, g * 512 + 3:g * 512 + 3 + 512], scalar1=convw[:, ct * 4 + 3:ct * 4 + 4],
                                                                    scalar2=convb[:, ct:ct + 1], op0=ALU.mult, op1=ALU.add),
                     reads=['raw%d_%d' % (ct, g), 'convw', 'convb'], writes=['cacc%d' % g])
            for tap in range(3):
                for g in gs_:
                    acc = cacc[g]
                    last = (tap == 2)
                    out_ap = qkT[:, ct, g * 512:(g + 1) * 512] if last else acc[:, :]
                    P.op('dve', lambda e, g=g, acc=acc, tap=tap, out_ap=out_ap: e.scalar_tensor_tensor(out=out_ap, in0=raw[:, ct, g * 512 + tap:g * 512 + tap + 512],
                                                                                                       scalar=convw[:, ct * 4 + tap:ct * 4 + tap + 1], in1=acc[:, :],
                                                                                                       op0=ALU.mult, op1=ALU.add),
                         reads=['raw%d_%d' % (ct, g), 'rawpad', 'convw', 'cacc%d' % g] + (['raw%d_%d' % (ct, g - 1)] if g > 0 else []),
                         writes=(['qkTpre%d_%d' % (ct, g)] if last else ['cacc%d' % g]))

    def conv_silu(ct):
        P.op('act', lambda e: e.activation(out=qkT[:, ct, :], in_=qkT[:, ct, :], func=AF.Silu),
             reads=['qkTpre%d_%d' % (ct, g) for g in range(4)], writes=['qkT%d_%d' % (ct, g) for g in range(4)])

    for grp in range(2):
        w, wk = pre_w[grp]
        for cl in range(4):
            ct = grp * 4 + cl
            for g in range(4):
                b = next_bank(0, 4)
                proj_fm(w, wk, cl, g, b)
                evac_copy(raw[:, ct, 3 + g * 512:3 + (g + 1) * 512], PS[:, b, :], ['ps%d' % b], ['raw%d_%d' % (ct, g)], force_act=True)
            if ct >= 1:
                conv_tile(ct - 1)
    w, wk = load_w(C_VM, 512)
    for t in range(NT):
        b = next_bank(0, 4)
        proj_tm(w, wk, t, 512, PS[:, b, :], 'ps%d' % b)
        evac_copy(vaug[:, t, :, 0:128], PS4[:, b, :, :], ['ps%d' % b], ['vaug%d' % t], force_act=True)
    conv_tile(7)
    for t in range(NT):
        proj_tm(wif, 'wif', t, 8, PS[:, 6, t * 8:(t + 1) * 8], 'ps6')
    P.op('dve', lambda e: e.tensor_tensor(out=fi[:, :], in0=PS[:, 6, 0:128], in1=bif[:, :], op=ALU.add), reads=['ps6', 'bif'], writes=['fi'])
    for n in range(2):
        b = next_bank(0, 4)
        for kc in range(8):
            P.op('pe', lambda e, n=n, kc=kc, b=b: e.matmul(PS[:, b, :], screp[:, kc, :], wg[:, kc, n * 512:(n + 1) * 512], start=(kc == 0), stop=(kc == 7)),
                 reads=['wg%d' % kc, 'screp%d' % kc], writes=['ps%d' % b])
        P.op('dve', lambda e, n=n, b=b: e.tensor_tensor(out=gate_bc[:, n * 512:(n + 1) * 512], in0=PS[:, b, :], in1=gate_bc[:, n * 512:(n + 1) * 512], op=ALU.add),
             reads=['ps%d' % b, 'gate_bc'], writes=['gate_bc', 'gate_done%d' % n])
    w_o, wk_o = load_w(C_OM, 512)
    w_z, wk_z = load_w(C_ZM, 512)
    ozg = []
    zi_c = [0]

    def mk_o(cl, g, idx):
        def f():
            if idx % 4 == 1:
                conv_silu(idx // 4)
            b = next_bank(0, 4)
            proj_fm(w_o, wk_o, cl, g, b)
            P.op('act', lambda e: e.activation(out=gT[:, cl, g * 512:(g + 1) * 512], in_=PS[:, b, :], func=AF.Sigmoid),
                 reads=['ps%d' % b, 'gate_done0', 'gate_done1'], writes=['gT%d_%d' % (cl, g)])
        return f

    def mk_z(cl, g, idx):
        def f():
            if idx % 4 == 1:
                conv_silu(4 + idx // 4)
            b = next_bank(0, 4)
            proj_fm(w_z, wk_z, cl, g, b)
            sz = szt[zi_c[0] % 2]
            szk = 'szt%d' % (zi_c[0] % 2)
            zi_c[0] += 1
            P.op('act', lambda e: e.activation(out=sz[:, :], in_=PS[:, b, :], func=AF.Silu), reads=['ps%d' % b], writes=[szk])
            P.op('dve', lambda e: e.scalar_tensor_tensor(out=gT[:, cl, g * 512:(g + 1) * 512], in0=gT[:, cl, g * 512:(g + 1) * 512],
                                                         scalar=gml[:, cl:cl + 1], in1=sz[:, :], op0=ALU.mult, op1=ALU.mult),
                 reads=['gT%d_%d' % (cl, g), szk, 'gml'], writes=['gT%d_%d' % (cl, g)])
        return f

    for cl in range(4):
        for g in range(4):
            ozg.append(mk_o(cl, g, cl * 4 + g))
    for cl in range(4):
        for g in range(4):
            ozg.append(mk_z(cl, g, cl * 4 + g))

    def emit_oz(n):
        for _ in range(n):
            if ozg:
                ozg.pop(0)()

    P.op('act', lambda e: e.activation(out=nl3[:, :, :], in_=fi3[:, :, 4:8], func=AF.Exp, scale=-1.0), reads=['fi'], writes=['nl'])
    P.op('act', lambda e: e.activation(out=nl[:, :], in_=nl[:, :], func=AF.Ln, bias=1.0), reads=['nl'], writes=['nl'])
    P.op('dve', lambda e: e.memset(linc[:, 0:4], 0.0), writes=['linc'])
    for t in range(16):
        P.op('dve', lambda e, t=t: e.tensor_tensor(out=linc[:, (t + 1) * 4:(t + 2) * 4], in0=linc[:, t * 4:(t + 1) * 4], in1=nl[:, t * 4:(t + 1) * 4], op=ALU.add),
             reads=['linc', 'nl'], writes=['linc'])
    emit_oz(5)
    P.op('pe', lambda e: e.matmul(PS[:, 7, 0:64], U[:, :], nl[:, :], start=True, stop=False), reads=['U', 'nl'], writes=['ps7'])
    P.op('pe', lambda e: e.matmul(PS[:, 7, 0:64], onesf[:, :], linc[:, 0:64], start=False, stop=True), reads=['onesf', 'linc'], writes=['ps7'])
    P.op('dve', lambda e: e.tensor_copy(NF[:, :], PS[:, 7, 0:64]), reads=['ps7'], writes=['NF'])
    P.op('dve', lambda e: e.tensor_tensor(out=gg3[:, :, :], in0=fi3[:, :, 0:4], in1=NF3[:, :, :], op=ALU.add), reads=['fi', 'NF'], writes=['gg'])
    emit_oz(3)
    P.op('pe', lambda e: e.transpose(PS[0:64, 7, 128:256], gg[:, 0:64], identf[:, :]), reads=['gg', 'identf'], writes=['ps7b'])
    P.op('dve', lambda e: e.tensor_reduce(out=tmax[0:64, 0:1], in_=PS[0:64, 7, 128:256], op=ALU.max, axis=mybir.AxisListType.X), reads=['ps7b'], writes=['tmax'])
    emit_oz(3)
    P.op('pe', lambda e: e.transpose(PS[0:1, 7, 256:320], tmax[0:64, 0:1], identf[0:64, 0:64]), reads=['tmax', 'identf'], writes=['ps7c'])
    P.op('dve', lambda e: e.tensor_copy(trow[0:1, :], PS[0:1, 7, 256:320]), reads=['ps7c'], writes=['trow'])
    P.op('dve', lambda e: e.memset(Rrow[0:1, 0:4], 0.0), writes=['Rrow'])
    for t in range(16):
        P.op('dve', lambda e, t=t: e.tensor_tensor(out=Rrow[0:1, (t + 1) * 4:(t + 2) * 4], in0=Rrow[0:1, t * 4:(t + 1) * 4], in1=trow[0:1, t * 4:(t + 1) * 4], op=ALU.max),
             reads=['Rrow', 'trow'], writes=['Rrow'])
    emit_oz(6)
    P.op('pe', lambda e: e.matmul(PS[:, 7, 320:388], onesf[0:1, 0:128], Rrow[0:1, 0:68], start=True, stop=True), reads=['onesf', 'Rrow'], writes=['ps7d'])
    emit_oz(4)
    P.op('dve', lambda e: e.tensor_copy(Rb[:, :], PS[:, 7, 320:388]), reads=['ps7d'], writes=['Rb'])
    P.op('dve', lambda e: e.tensor_tensor(out=u12[:, 0, :], in0=gg[:, :], in1=Rb[:, 0:64], op=ALU.subtract), reads=['gg', 'Rb'], writes=['u12'])
    P.op('dve', lambda e: e.tensor_tensor(out=u12[:, 1, :], in0=gg[:, :], in1=Rb[:, 4:68], op=ALU.subtract), reads=['gg', 'Rb'], writes=['u12'])
    P.op('dve', lambda e: e.tensor_tensor(out=u12[:, 2, :], in0=NF[:, :], in1=Rb[:, 0:64], op=ALU.subtract), reads=['NF', 'Rb'], writes=['u12'])
    P.op('dve', lambda e: e.tensor_tensor(out=u12[:, 3, :], in0=Rb[:, 0:64], in1=Rb[:, 4:68], op=ALU.subtract), reads=['Rb'], writes=['u12'])
    P.op('act', lambda e: e.activation(out=u12[:, 0:2, :], in_=u12[:, 0:2, :], func=AF.Exp, bias=LN_S), reads=['u12'], writes=['u12'])
    P.op('act', lambda e: e.activation(out=u12[:, 2:4, :], in_=u12[:, 2:4, :], func=AF.Exp), reads=['u12'], writes=['u12'])
    emit_oz(len(ozg))
    preC = [load_w(C_QB, 512), load_w(C_KB, 512)]
    if 'qkT' in dumps:
        add_dump('qkT', qkT[:, :, :], [128, 8, S], BF16, ['qkT%d_%d' % (a, b) for a in range(8) for b in range(4)])
    if 'u12' in dumps:
        add_dump('u12', u12[:, :, :], [128, 4, 64], F32, 'u12')
    if 'gT' in dumps:
        add_dump('gT', gT[:, :, :], [128, 4, S], BF16, ['gT%d_%d' % (a, b) for a in range(4) for b in range(4)])
    A.release(mB2)
    if upto <= 2:
        P.emit()
        return nc, dump_d

    P.barrier(exclude=('wbuf0', 'wbuf1'))
    stil = [A.alloc("stil%d" % i, [128, 128], BF16) for i in range(2)]
    ktil = [A.alloc("ktil%d" % i, [128, 128], BF16) for i in range(2)]
    Cf = [A.alloc("Cf%d" % i, [128, 130], F32) for i in range(4)]
    Cb = [A.alloc("Cb%d" % i, [128, 130], BF16) for i in range(4)]
    Asb = A.alloc("Asb", [128, NT, 4, 130], F32)
    Bsb = A.alloc("Bsb", [128, 64], F32)
    ssqA = A.alloc("ssqA", [128, 64], F32)
    junkA = [A.alloc("junkA%d" % i, [128, 128], BF16) for i in range(2)]
    Af = [A.alloc("Af%d" % i, [128, 130], F32) for i in range(2)]
    sm = A.alloc("sm", [128, 4, 64], F32)
    hnb = [A.alloc("hnb%d" % i, [128, 128], BF16) for i in range(4)]
    stepsB = [(t, h) for t in range(NT) for h in range(4)]

    def stage1(i):
        t, h = stepsB[i]
        par = i % 2
        col = t * 4 + h
        tl = slice(t * 128, (t + 1) * 128)
        qk_q = 'qkT%d_%d' % (h, t // 4)
        qk_k = 'qkT%d_%d' % (4 + h, t // 4)
        P.op('pe', lambda e: e.matmul(PS[:, par, 0:128], qkT[:, 4 + h, tl], qkT[:, h, tl], start=True, stop=True),
             reads=[qk_q, qk_k], writes=['ps%d' % par])
        P.op('dve', lambda e: e.scalar_tensor_tensor(out=stil[par][:, :], in0=PS[:, par, 0:128], scalar=u12[:, 0, col:col + 1], in1=tri01[:, :],
                                                     op0=ALU.mult, op1=ALU.mult),
             reads=['ps%d' % par, 'u12', 'tri01'], writes=['stil%d' % par])
        if t < NT - 1:
            P.op('pe', lambda e: e.transpose(PSB[:, 4 + par, 0:128], qkT[:, 4 + h, tl], identb[:, :]),
                 reads=[qk_k, 'identb'], writes=['ps%d' % (4 + par)])
            P.op('act', lambda e: e.activation(out=ktil[par][:, :], in_=PSB[:, 4 + par, 0:128], func=AF.Copy, scale=u12[:, 1, col:col + 1]),
                 reads=['ps%d' % (4 + par), 'u12'], writes=['ktil%d' % par])

    def stage2(i):
        t, h = stepsB[i]
        par = i % 2
        col = t * 4 + h
        tl = slice(t * 128, (t + 1) * 128)
        qk_q = 'qkT%d_%d' % (h, t // 4)
        P.op('pe', lambda e: e.matmul(PS[:, 2 + par, 0:129], stil[par][:, :], vaug[:, t, h, 0:129], start=True, stop=(t == 0)),
             reads=['stil%d' % par, 'vaug%d' % t, 'vones'], writes=['ps%d' % (2 + par)])
        if t > 0:
            P.op('pe', lambda e: e.matmul(PS[:, 2 + par, 0:129], qkT[:, h, tl], Cb[h][:, 0:129], start=False, stop=True),
                 reads=[qk_q, 'Cb%d' % h], writes=['ps%d' % (2 + par)])
        if t < NT - 1:
            P.op('pe', lambda e: e.matmul(PS[:, 6 + par, 0:129], ktil[par][:, :], vaug[:, t, h, 0:129], start=True, stop=True),
                 reads=['ktil%d' % par, 'vaug%d' % t, 'vones'], writes=['ps%d' % (6 + par)])
            if t == 0:
                P.op('dve', lambda e: e.tensor_copy(Cf[h][:, 0:129], PS[:, 6 + par, 0:129]), reads=['ps%d' % (6 + par)], writes=['Cf%d' % h])
            else:
                P.op('dve', lambda e: e.scalar_tensor_tensor(out=Cf[h][:, 0:129], in0=Cf[h][:, 0:129], scalar=u12[:, 3, col:col + 1],
                                                             in1=PS[:, 6 + par, 0:129], op0=ALU.mult, op1=ALU.add),
                     reads=['ps%d' % (6 + par), 'u12', 'Cf%d' % h], writes=['Cf%d' % h])
            P.op('pool', lambda e: e.tensor_copy(Cb[h][:, 0:129], Cf[h][:, 0:129]), reads=['Cf%d' % h], writes=['Cb%d' % h])
        P.op('dve', lambda e: e.tensor_copy(Asb[:, t, h, 0:129], PS[:, 2 + par, 0:129]), reads=['ps%d' % (2 + par)], writes=['Asb%d_%d' % (t, h)])
        P.op('act', lambda e: e.activation(out=junkA[par][:, :], in_=Asb[:, t, h, 0:128], func=AF.Square, accum_out=ssqA[:, col:col + 1]),
             reads=['Asb%d_%d' % (t, h)], writes=['ssqA%d' % col, 'junkA%d' % par])

    for i in range(len(stepsB) + 1):
        if i < len(stepsB):
            stage1(i)
        if i >= 1:
            stage2(i - 1)
    allB = ['Asb%d_%d' % (t_, h_) for t_ in range(NT) for h_ in range(4)]
    allS = ['ssqA%d' % c for c in range(64)]
    Bsb3 = Bsb.reshape([128, NT, 4])
    P.op('dve', lambda e: e.tensor_copy(Bsb3[:, :, :], Asb[:, :, :, 128]), reads=allB, writes=['Bsb'])
    P.op('dve', lambda e: e.scalar_tensor_tensor(out=sm[:, 0, :], in0=Bsb[:, :], scalar=-1.0, in1=Bsb[:, :], op0=ALU.mult, op1=ALU.max), reads=['Bsb'], writes=['sm0'])
    P.op('dve', lambda e: e.tensor_tensor(out=sm[:, 0, :], in0=sm[:, 0, :], in1=u12[:, 2, :], op=ALU.max), reads=['u12', 'sm0'], writes=['sm0'])
    P.op('dve', lambda e: e.reciprocal(sm[:, 1, :], sm[:, 0, :]), reads=['sm0'], writes=['sm1'])
    P.op('dve', lambda e: e.tensor_tensor(out=sm[:, 2, :], in0=sm[:, 1, :], in1=sm[:, 1, :], op=ALU.mult), reads=['sm1'], writes=['sm2'])
    P.op('dve', lambda e: e.tensor_tensor(out=sm[:, 2, :], in0=sm[:, 2, :], in1=ssqA[:, :], op=ALU.mult), reads=['sm2'] + allS, writes=['sm2'])
    P.op('act', lambda e: e.activation(out=sm[:, 2, :], in_=sm[:, 2, :], func=AF.Sqrt, scale=1.0 / 128.0, bias=EPS), reads=['sm2'], writes=['sm2'])
    P.op('dve', lambda e: e.reciprocal(sm[:, 3, :], sm[:, 2, :]), reads=['sm2'], writes=['sm3'])
    P.op('dve', lambda e: e.tensor_tensor(out=sm[:, 3, :], in0=sm[:, 3, :], in1=sm[:, 1, :], op=ALU.mult), reads=['sm3', 'sm1'], writes=['sm3'])
    cnt = 0
    for g in range(4):
        for h in range(4):
            b = cnt % 2
            cnt += 1
            for j in range(4):
                t = 4 * g + j
                col = t * 4 + h
                sl = (cnt * 4 + j) % 4
                if j % 2 == 0:
                    P.op('act', lambda e, sl=sl, t=t, h=h, col=col: e.activation(out=hnb[sl][:, :], in_=Asb[:, t, h, 0:128], func=AF.Copy, scale=sm[:, 3, col:col + 1]),
                         reads=['Asb%d_%d' % (t, h), 'sm3'], writes=['hnb%d' % sl])
                else:
                    P.op('pool', lambda e, sl=sl, t=t, h=h, col=col: e.tensor_scalar(out=hnb[sl][:, :], in0=Asb[:, t, h, 0:128], scalar1=sm[:, 3, col:col + 1], scalar2=1.0,
                                                                                     op0=ALU.mult, op1=ALU.mult),
                         reads=['Asb%d_%d' % (t, h), 'sm3'], writes=['hnb%d' % sl])
                P.op('pe', lambda e, sl=sl, b=b, j=j: e.transpose(PSB[:, b, j * 128:(j + 1) * 128], hnb[sl][:, :], identb[:, :]),
                     reads=['hnb%d' % sl, 'identb'], writes=['ps%d' % b])
            P.op('dve', lambda e, b=b, g=g, h=h: e.tensor_tensor(out=catT[:, h, g * 512:(g + 1) * 512], in0=PSB[:, b, 0:512], in1=gT[:, h, g * 512:(g + 1) * 512], op=ALU.mult),
                 reads=['ps%d' % b, 'gT%d_%d' % (h, g)], writes=['catT%d_%d' % (h, g)])
    if 'catA' in dumps:
        add_dump('catA', catT[:, 0:4, :], [128, 4, S], BF16, ['catT%d_%d' % (a, b) for a in range(4) for b in range(4)])
    if 'Asb' in dumps:
        add_dump('Asb', Asb[:, :, :, :], [128, NT, 4, 128], BF16, ['Asb%d_%d' % (a, b) for a in range(NT) for b in range(4)])
    if 'sm' in dumps:
        add_dump('sm', sm[:, :, :], [128, 4, 64], F32, ['sm0', 'sm1', 'sm2', 'sm3'])
    A.release(mB)
    if upto <= 3:
        P.emit()
        return nc, dump_d

    P.barrier()
    mC = A.mark()
    PS8 = PS.reshape([128, 8, 8, 64])
    PSg = PS.reshape([128, 8, 64, 8])
    A.off = (A.off + 63) // 64 * 64
    Qaug_off = A.off
    Qaug = A.alloc("Qaug", [128, 8, S], BF16)
    Kaug = A.alloc("Kaug", [128, 8, S], BF16)
    Kaug4 = Kaug.reshape([128, 8, 8, 256])
    vb = A.alloc("vb", [128, NT, 8, 72], BF16)
    vb_end = A.off
    szb = A.alloc("szb", [128, 4, S], BF16)
    A.off = (A.off + 63) // 64 * 64
    qst_off = A.off
    qst = [A.alloc("qst%d" % i, [128, S], BF16) for i in range(2)]
    sel = A.alloc("sel", [128, 8, 64], BF16)
    kmf = A.alloc("kmf", [128, 64], F32)
    kmb = A.alloc("kmb", [128, 8, 8], BF16)
    gsb = A.alloc("gsb", [128, 8, 8, 8], F32)
    t8 = A.alloc("t8", [128, 8, 8, 8], F32)
    mq = A.alloc("mq", [128, 8, 8, 8], BF16)
    mT = A.alloc("mT", [128, 1024], BF16)
    kmb2 = kmb.reshape([128, 64])

    P.dma('sp', Kaug[64:75, :, :], kaug_d[:, :, :], writes=['Kconst'], group='cconst')
    P.dma('sp', Qaug[72:75, :, :], qaug_d[:, :, :], writes=['Qconst'], group='cconst')
    P.dma('sp', sel[64:72, :, :], sel_d[:, :, :], writes=['sel'], group='cconst')
    P.op('pool', lambda e: e.memset(Qaug[64:72, :, 0:1024], 0.0), writes=['Qmzero'])
    P.op('pool', lambda e: e.memset(vb[:, :, :, 64:72], 0.0), writes=['vbones'])
    for h in range(8):
        P.op('pool', lambda e, h=h: e.memset(vb[:, :, h, 64 + h:65 + h], 1.0), reads=['vbones'], writes=['vbones'])
    P.op('pool', lambda e: e.memset(gsb[:, :, :, :], NEG), writes=['gsbinit'])
    sti = 0
    for which, c0, dst, nm, scl in (('q', C_QB, Qaug, 'Qd', 0.125), ('k', C_KB, Kaug, 'Kd', None)):
        w, wk = preC[0 if which == 'q' else 1]
        for cl in range(4):
            st = qst[sti % 2]
            stk = 'qst%d' % (sti % 2)
            sti += 1
            for g in range(4):
                b = next_bank(0, 4)
                proj_fm(w, wk, cl, g, b)
                evac_copy(st[:, g * 512:(g + 1) * 512], PS[:, b, :], ['ps%d' % b], [stk + '_%d' % g], scale=scl)
            sk = [stk + '_%d' % g for g in range(4)]
            P.dma('sp', dst[0:64, 2 * cl, :], st[0:64, :], reads=sk, writes=['%s%d' % (nm, 2 * cl)], group=nm)
            P.dma('sp', dst[0:64, 2 * cl + 1, :], st[64:128, :], reads=sk, writes=['%s%d' % (nm, 2 * cl + 1)], group=nm)
    for h in range(8):
        P.op('dve', lambda e, h=h: e.tensor_reduce(out=kmf[0:64, h * 8:(h + 1) * 8], in_=Kaug4[0:64, h, :, :], op=ALU.add, axis=mybir.AxisListType.X),
             reads=['Kd%d' % h], writes=['kmf%d' % h])
    P.op('dve', lambda e: e.tensor_scalar(out=kmb2[0:64, :], in0=kmf[0:64, :], scalar1=1.0 / 256.0, scalar2=None, op0=ALU.mult),
         reads=['kmf%d' % h for h in range(8)], writes=['kmb'])
    w, wk = load_w(C_VB, 512)
    for t in range(NT):
        b = next_bank(0, 4)
        proj_tm(w, wk, t, 512, PS[:, b, :], 'ps%d' % b)
        evac_copy(vb[:, t, :, 0:64], PS8[:, b, :, :], ['ps%d' % b], ['vb%d' % t], force_act=True)
    def gate_tile(qt):
        j = qt // 2
        i2 = qt % 2
        gb = 4 + i2
        for h in range(8):
            P.op('pe', lambda e, h=h: e.matmul(PS[:, gb, h * 8:(h + 1) * 8], Qaug[0:64, h, qt * 128:(qt + 1) * 128], kmb[0:64, h, :], start=True, stop=True),
                 reads=['Qd%d' % h, 'kmb'], writes=['ps%d' % gb])
        P.op('dve', lambda e: e.tensor_copy(gsb[:, qt - 8, :, 0:j], PSg[:, gb, 0:8, 0:j]), reads=['ps%d' % gb, 'gsbinit'], writes=['gsb%d' % qt])
        for h in range(8):
            P.op('dve', lambda e, h=h: e.max(t8[:, qt - 8, h, :], gsb[:, qt - 8, h, :]), reads=['gsb%d' % qt], writes=['t8_%d_%d' % (qt, h)])
            P.op('dve', lambda e, h=h: e.tensor_scalar(out=mq[:, qt - 8, h, :], in0=gsb[:, qt - 8, h, :], scalar1=t8[:, qt - 8, h, 2:3], scalar2=1.0,
                                                       op0=ALU.is_ge, op1=ALU.subtract),
                 reads=['gsb%d' % qt, 't8_%d_%d' % (qt, h)], writes=['mq%d' % qt])
        P.op('dve', lambda e: e.memset(mq[:, qt - 8, :, j:j + 1], 0.0), reads=['mq%d' % qt], writes=['mq%d' % qt])

    w, wk = load_w(C_ZB, 512)
    zi_ = 0
    for cl in range(4):
        for g in range(4):
            if zi_ % 2 == 0:
                gate_tile(8 + zi_ // 2)
            zi_ += 1
            b = next_bank(0, 4)
            proj_fm(w, wk, cl, g, b)
            P.op('act', lambda e, b=b, cl=cl, g=g: e.activation(out=szb[:, cl, g * 512:(g + 1) * 512], in_=PS[:, b, :], func=AF.Silu),
                 reads=['ps%d' % b], writes=['szb%d_%d' % (cl, g)])
    mqf = mq.reshape([128, 8, 64])
    qm_ops = []

    def mask_finish():
        for qt in range(8, 16):
            P.op('pe', lambda e, qt=qt: e.transpose(PSB[0:64, 7, (qt - 8) * 128:(qt - 7) * 128], mqf[:, qt - 8, :], identb[:, :]),
                 reads=['mq%d' % qt, 'identb'], writes=['ps7'])
        P.op('dve', lambda e: e.tensor_copy(mT[0:64, :], PSB[0:64, 7, 0:1024]), reads=['ps7'], writes=['mT'])
        for h in range(8):
            qm_ops.append(P.dma('sp', Qaug[64:72, h, 1024:2048], mT[8 * h:8 * h + 8, :], reads=['mT'], writes=['Qm%d' % h], group='Qm'))

    wout_v = wout_d.rearrange("(kc p) c -> p kc c", p=128)
    for n in range(2):
        P.dma('pool', wbufs[n][:, :, :], wout_v[:, :, n * 512:(n + 1) * 512], writes=['wbuf%d' % n])
    for n in range(2):
        for kc in range(8):
            P.op('pool', lambda e, n=n, kc=kc: e.tensor_tensor(out=wbufs[n][:, kc, :], in0=wbufs[n][:, kc, :], in1=gate_bc[:, n * 512:(n + 1) * 512], op=ALU.mult),
                 reads=['wbuf%d' % n, 'gate_bc'], writes=['wbuf%d' % n])
    cur = A.off
    A.off = hT_off
    pt = [A.alloc("pt%d" % i, [128, 2, 512], BF16) for i in range(3)]
    onum2 = [A.alloc("onum%d" % i, [128, 8, 512], BF16) for i in range(2)]
    dens = A.alloc("dens", [128, 512], F32)
    rdf = A.alloc("rdf", [128, 512], F32)
    ost = [A.alloc("ost%d" % i, [128, 512], BF16) for i in range(4)]
    assert A.off <= hT_off + 8 * S * 2
    assert cur - qst_off >= 16384, (cur, qst_off)
    A.off = qst_off
    bc = A.alloc("bc", [128, 8, 512], F32)
    A.off = cur
    stepsC = []
    for qc in range(4):
        for h in range(8):
            lst = [('pair', pr) for pr in range(2 * qc)] + [('diag', r) for r in range(4)]
            for n_, (kind, idx) in enumerate(lst):
                stepsC.append((qc, h, kind, idx, n_ == 0, n_ == len(lst) - 1))
    osti_ = [0]
    defer = []

    def tick(flush=False):
        for d in defer:
            d[0] -= 1
        while defer and (flush or defer[0][0] <= 0):
            defer.pop(0)[1]()

    def szb_mult(c, g):
        ck = 'catT%d_%d' % (4 + c, g)
        P.op('dve', lambda e: e.tensor_tensor(out=catT[:, 4 + c, g * 512:(g + 1) * 512], in0=catT[:, 4 + c, g * 512:(g + 1) * 512],
                                              in1=szb[:, c, g * 512:(g + 1) * 512], op=ALU.mult),
             reads=[ck + 'a', ck + 'b', 'szb%d_%d' % (c, g)], writes=[ck])

    def att_front(i):
        qc, h, kind, idx, isfirst, islast = stepsC[i]
        st_ = i % 3
        qs = slice(qc * 512, (qc + 1) * 512)
        rk = ['Qd%d' % h, 'Qconst', 'Qmzero', 'Kd%d' % h, 'Kconst'] + (['Qm%d' % h] if qc >= 2 else [])
        if kind == 'pair':
            for i2 in range(2):
                kt = 2 * idx + i2
                P.op('pe', lambda e, i2=i2, kt=kt: e.matmul(PS[:, 2 * st_ + i2, :], Kaug[0:75, h, kt * 128:(kt + 1) * 128], Qaug[0:75, h, qs], start=True, stop=True),
                     reads=rk, writes=['ps%d' % (2 * st_ + i2)])
            for i2 in range(2):
                P.op('act', lambda e, i2=i2: e.activation(out=pt[st_][:, i2, :], in_=PS[:, 2 * st_ + i2, :], func=AF.Exp),
                     reads=['ps%d' % (2 * st_ + i2)], writes=['pt%d_%d' % (st_, i2)])
        else:
            r = idx
            kt = 4 * qc + r
            c0 = 128 * r
            bk = 2 * st_
            P.op('pe', lambda e: e.matmul(PS[:, bk, c0:512], Kaug[0:75, h, kt * 128:(kt + 1) * 128], Qaug[0:75, h, qc * 512 + c0:(qc + 1) * 512],
                                          start=True, stop=False, skip_group_check=True),
                 reads=rk, writes=['ps%d' % bk])
            P.op('pe', lambda e: e.matmul(PS[:, bk, c0:c0 + 128], identb[:, :], cmask[:, :], start=False, stop=True, skip_group_check=True),
                 reads=['identb', 'cmask'], writes=['ps%d' % bk])
            P.op('act', lambda e: e.activation(out=pt[st_][:, 0, c0:512], in_=PS[:, bk, c0:512], func=AF.Exp),
                 reads=['ps%d' % bk], writes=['pt%d_0' % st_])

    def att_back(i):
        qc, h, kind, idx, isfirst, islast = stepsC[i]
        st_ = i % 3
        qs = slice(qc * 512, (qc + 1) * 512)
        ob = 6 + h % 2
        okey = 'ps%d' % ob
        if kind == 'pair':
            for i2 in range(2):
                kt = 2 * idx + i2
                P.op('pe', lambda e, i2=i2, kt=kt: e.matmul(PS[0:72, ob, :], vb[:, kt, h, 0:72], pt[st_][:, i2, :],
                                                            start=(isfirst and i2 == 0), stop=False, skip_group_check=True),
                     reads=['pt%d_%d' % (st_, i2), 'vb%d' % kt, 'vbones'], writes=[okey])
        else:
            r = idx
            kt = 4 * qc + r
            c0 = 128 * r
            P.op('pe', lambda e: e.matmul(PS[0:72, ob, c0:512], vb[:, kt, h, 0:72], pt[st_][:, 0, c0:512],
                                          start=isfirst, stop=(r == 3), skip_group_check=True),
                 reads=['pt%d_0' % st_, 'vb%d' % kt, 'vbones'], writes=[okey])
        if not islast:
            return
        onum = onum2[qc % 2]
        onk = 'onum%d_' % (qc % 2)
        P.op('dve', lambda e: e.tensor_copy(onum[0:64, h, :], PS[0:64, ob, :]), reads=[okey], writes=[onk + '%d' % h])
        if h == 0:
            P.op('dve', lambda e: e.tensor_copy(dens[64:72, :], PS[64:72, ob, :]), reads=[okey], writes=['dens'])
        else:
            P.op('dve', lambda e: e.tensor_tensor(out=dens[64:72, :], in0=dens[64:72, :], in1=PS[64:72, ob, :], op=ALU.add), reads=[okey, 'dens'], writes=['dens'])
        tick()
        if h % 4 != 3:
            return
        gi = h // 4
        hi_ = 64 + 4 * (gi + 1)
        P.op('dve', lambda e: e.reciprocal(rdf[64:hi_, :], dens[64:hi_, :]), reads=['dens'], writes=['rdf'])
        P.dma('sp', scr_d[4 * gi:4 * gi + 4, :], rdf[64 + 4 * gi:68 + 4 * gi, :], reads=['rdf'], writes=['scr%d' % gi], extra=list(qm_ops))
        for hh in range(4 * gi, 4 * gi + 4):
            P.dma('sp', bc[0:64, hh:hh + 1, :], scr_d[hh:hh + 1, :].partition_broadcast(64), reads=['scr%d' % gi], writes=['bc%d' % hh], extra=list(qm_ops))

        def part2(qc=qc, gi=gi, qs=qs, onum=onum, onk=onk):
            for hh in range(4 * gi, 4 * gi + 4):
                ck = 'catT%d_%d' % (4 + hh // 2, qc)
                if hh % 2 == 0:
                    P.op('dve', lambda e, hh=hh: e.tensor_tensor(out=catT[0:64, 4 + hh // 2, qs], in0=onum[0:64, hh, :], in1=bc[0:64, hh, :], op=ALU.mult),
                         reads=['bc%d' % hh, onk + '%d' % hh], writes=[ck + 'a'])
                else:
                    o_ = ost[osti_[0] % 4]
                    ok_ = 'ost%d' % (osti_[0] % 4)
                    osti_[0] += 1
                    P.op('dve', lambda e, hh=hh, o_=o_: e.tensor_tensor(out=o_[0:64, :], in0=onum[0:64, hh, :], in1=bc[0:64, hh, :], op=ALU.mult),
                         reads=['bc%d' % hh, onk + '%d' % hh], writes=[ok_])
                    P.dma('sp', catT[64:128, 4 + hh // 2, qs], o_[0:64, :], reads=[ok_], writes=[ck + 'b'], group='catTb%d_%d' % (qc, gi))

        def part3(qc=qc, gi=gi):
            szb_mult(2 * gi, qc)
            szb_mult(2 * gi + 1, qc)

        defer.append([2, part2])
        defer.append([4, part3])

    for i in range(len(stepsC) + 1):
        if i < len(stepsC):
            att_front(i)
        if i >= 1:
            att_back(i - 1)
        if i == 10:
            mask_finish()
    assert len(qm_ops) == 8
    last_att_pe = max(i for i, o in enumerate(P.ops) if o['eng'] == 'pe' and not o['dma'])
    if 'catB' in dumps:
        add_dump('catB', catT[:, 4:8, :], [128, 4, S], BF16, ['catT%d_%d' % (a, b) for a in range(4, 8) for b in range(4)])
    A.release(mC)
    if upto <= 4:
        P.emit()
        return nc, dump_d

    P.fence([last_att_pe])
    A.off = Qaug_off
    gfin = A.alloc("gfin", [128, D], F32)
    xr = [A.alloc("xr%d" % i, [128, D], F32) for i in range(4)]
    pre = A.alloc("pre", [128, 8, D], F32)
    ot = [A.alloc("ot%d" % i, [128, D], F32) for i in range(6)]
    ssqo = A.alloc("ssqo", [128, NT], F32)
    rso = A.alloc("rso", [128, NT], F32)
    jk = [A.alloc("jk%d" % i, [128, D], BF16) for i in range(2)]
    assert A.off <= vb_end, (A.off, vb_end)
    ld(gfin[:, :], gfin_d[:, :], 'gfin', group='gfin')

    def fin_tile(tt):
        g4 = tt // 4
        ok_ = 'ot%d' % (tt % 6)
        P.op('dve', lambda e: e.scalar_tensor_tensor(out=ot[tt % 6][:, :], in0=pre[:, tt % 8, :], scalar=rso[:, tt:tt + 1], in1=gfin[:, :], op0=ALU.mult, op1=ALU.mult),
             reads=['pre%d_0' % (tt % 8), 'pre%d_1' % (tt % 8), 'rso%d' % g4, 'gfin'], writes=[ok_])
        P.dma('pool', out_d[tt * 128:(tt + 1) * 128, :], ot[tt % 6][:, :], reads=[ok_], writes=['out'], final=True)

    pend = []

    def x_reload(t):
        P.dma('act', xr[t % 4][:, :], x_d[t * 128:(t + 1) * 128, :], writes=['xr%d' % (t % 4)])

    for t in range(3):
        x_reload(t)
    for t in range(NT):
        xk = 'xr%d' % (t % 4)
        if t + 3 < NT:
            x_reload(t + 3)
        for n in range(2):
            b = next_bank(0, 4)
            for kc in range(8):
                P.op('pe', lambda e, b=b, kc=kc, t=t, n=n: e.matmul(PS[:, b, :], catT[:, kc, t * 128:(t + 1) * 128], wbufs[n][:, kc, :],
                                                                   start=(kc == 0), stop=(kc == 7)),
                     reads=['wbuf%d' % n, 'catT%d_%d' % (kc, t // 4)], writes=['ps%d' % b])
            P.op('dve', lambda e, b=b, t=t, n=n: e.tensor_tensor(out=pre[:, t % 8, n * 512:(n + 1) * 512], in0=PS[:, b, :], in1=xr[t % 4][:, n * 512:(n + 1) * 512], op=ALU.add),
                 reads=['ps%d' % b, xk], writes=['pre%d_%d' % (t % 8, n)])
        P.op('act', lambda e, t=t: e.activation(out=jk[t % 2][:, :], in_=pre[:, t % 8, :], func=AF.Square, accum_out=ssqo[:, t:t + 1]),
             reads=['pre%d_0' % (t % 8), 'pre%d_1' % (t % 8)], writes=['ssqo%d' % t, 'jk%d' % (t % 2)])
        if pend:
            fin_tile(pend.pop(0))
        if t == 2 or t == 5:
            tick()
            tick()
        if t == 8:
            tick(flush=True)
        if t % 4 == 3:
            g4 = t // 4
            P.op('act', lambda e, g4=g4: e.activation(out=rso[:, g4 * 4:(g4 + 1) * 4], in_=ssqo[:, g4 * 4:(g4 + 1) * 4], func=AF.Sqrt, scale=1.0 / D, bias=EPS),
                 reads=['ssqo%d' % tt for tt in range(g4 * 4, g4 * 4 + 4)], writes=['rso%d' % g4])
            P.op('dve', lambda e, g4=g4: e.reciprocal(rso[:, g4 * 4:(g4 + 1) * 4], rso[:, g4 * 4:(g4 + 1) * 4]), reads=['rso%d' % g4], writes=['rso%d' % g4])
            pend.extend(range(g4 * 4, g4 * 4 + 4))
    while pend:
        fin_tile(pend.pop(0))
    P.emit()
    return nc, dump_d


def _bf(a):
    return np.ascontiguousarray(np.asarray(a, dtype=np.float32)).astype(ml_dtypes.bfloat16)


def host_consts():
    c = {}
    eye = np.eye(128, dtype=np.float32)
    c["identb"] = _bf(eye)
    c["identf"] = eye
    up = (np.arange(128)[:, None] <= np.arange(128)[None, :]).astype(np.float32)
    c["U"] = up
    c["tri01"] = _bf(up)
    c["cmask"] = _bf(np.where(np.arange(128)[:, None] > np.arange(128)[None, :], -30000.0, 0.0))
    pos = np.arange(S)
    kaug = np.zeros((11, 8, S), np.float32)
    qaug = np.zeros((3, 8, S), np.float32)
    for h in range(8):
        slope = 2.0 ** (-(h + 1))
        for r in range(8):
            kaug[r, h] = np.where(pos // 256 == r, 32768.0, 0.0)
        kaug[8, h] = 1.0
        kaug[9, h] = slope * 128.0 * (pos // 128)
        kaug[10, h] = slope * (pos % 128)
        qaug[0, h] = -slope * pos
        qaug[1, h] = 1.0
        qaug[2, h] = 1.0
    c["kaugc"] = _bf(kaug)
    c["qaugc"] = _bf(qaug)
    sel = np.zeros((8, 8, 64), np.float32)
    for h in range(8):
        sel[h, h, :] = 1.0
    c["selc"] = _bf(sel)
    return c


def host_inputs(inp, b, consts=None):
    f = lambda a: np.ascontiguousarray(np.asarray(a, dtype=np.float32))
    m = dict(consts if consts is not None else host_consts())
    m["x"] = f(inp["x"][b])
    m["cT"] = f(np.asarray(inp["c"][b]).reshape(8, 128).T)
    m["w_ada"] = f(inp["w_ada"][0])
    bada = np.asarray(inp["b_ada"][0])
    m["bss"] = f(bada[:2048].reshape(16, 128).T)
    m["bgate"] = f(np.broadcast_to(bada[2048:][None, :], (128, D)))
    m["gnT"] = f(np.asarray(inp["g_norm"][0]).reshape(8, 128).T)
    m["w_in"] = f(inp["w_in"][0])
    m["convw"] = f(np.asarray(inp["conv_w"][0]).reshape(4, 8, 128).transpose(2, 1, 0).reshape(128, 32))
    m["convb"] = f(np.asarray(inp["conv_b"][0]).reshape(8, 128).T)
    bi = np.asarray(inp["b_igate"][0]); bfv = np.asarray(inp["b_fgate"][0])
    m["bif"] = f(np.broadcast_to(np.tile(np.concatenate([bi, bfv]), NT)[None, :], (128, 128)))
    m["gml"] = f(np.asarray(inp["g_mlstm_head"][0]).reshape(4, 128).T)
    m["w_out"] = f(inp["w_out"][0])
    m["gfin"] = f(np.broadcast_to(np.asarray(inp["g_final"])[None, :], (128, D)))
    return m


_CACHE = {}


def kernel(**inputs):
    consts = host_consts()
    in_maps = [host_inputs(inputs, b, consts) for b in range(8)]
    nc, _ = build_program()
    res = run_bass_kernel_spmd(nc, in_maps, core_ids=list(range(8)))
    out = np.stack([np.asarray(r["out"], dtype=np.float32) for r in res.results], axis=0)
    return out
```

```python
import numpy as np
import ml_dtypes
import concourse.bass as bass
import concourse.mybir as mybir
from concourse.bass_utils import run_bass_kernel_spmd

F32 = mybir.dt.float32
BF16 = mybir.dt.bfloat16
AF = mybir.ActivationFunctionType
ALU = mybir.AluOpType

_COMPUTE = ('pe', 'act', 'dve', 'pool')


class Prog:
    def __init__(self, nc):
        self.nc = nc
        self.ops = []
        self.last_w = {}
        self.readers = {}
        self.eng_sem = {}
        self.key_sem = {}
        self.final_keys = []
        self.bar = None

    def barrier(self, exclude=()):
        lasts = {}
        for i, o in enumerate(self.ops):
            key = ('k', o['semkey']) if o['dma'] else ('e', o['eng'])
            if o['dma'] and o['semkey'] in exclude:
                continue
            lasts[key] = i
        self.bar = (set(lasts.values()), set())

    def fence(self, dep_ops):
        self.bar = (set(dep_ops), set())

    def _add(self, eng, fn, reads, writes, is_dma, semkey=None, extra=()):
        i = len(self.ops)
        deps = set(extra)
        psr = [k for k in reads if isinstance(k, str) and k.startswith('ps')]
        if psr:
            reads = [k for k in reads if k not in psr]
            writes = list(writes) + psr
        if self.bar is not None and eng not in self.bar[1]:
            deps |= self.bar[0]
            self.bar[1].add(eng)
        for k in reads:
            if k in self.last_w:
                deps.add(self.last_w[k])
        for k in writes:
            if k in self.last_w:
                deps.add(self.last_w[k])
            for r in self.readers.get(k, ()):
                deps.add(r)
        deps.discard(i)
        self.ops.append(dict(eng=eng, fn=fn, deps=sorted(deps), dma=is_dma, semkey=semkey, sig=is_dma))
        for k in reads:
            self.readers.setdefault(k, []).append(i)
        for k in writes:
            self.last_w[k] = i
            self.readers[k] = []
        return i

    def op(self, eng, fn, reads=(), writes=()):
        return self._add(eng, fn, list(reads), list(writes), False)

    def dma(self, queue, out, in_, reads=(), writes=(), final=False, group=None, extra=()):
        writes = list(writes)
        semkey = writes[0] if group is None else ('G', group)
        if final:
            self.final_keys.append(semkey)
        return self._add(queue, lambda e: e.dma_start(out=out, in_=in_), list(reads), writes, True, semkey, extra=extra)

    def emit(self):
        nc = self.nc
        ops = self.ops
        for o in ops:
            for d in o['deps']:
                p = ops[d]
                if p['dma']:
                    continue
                if p['eng'] == 'pe' and o['eng'] == 'pe' and not o['dma']:
                    continue
                p['sig'] = True
        cnt = {}
        for o in ops:
            if o['dma']:
                k = ('k', o['semkey'])
                cnt[k] = cnt.get(k, 0) + 16
                o['tok'] = (k, cnt[k])
            elif o['sig']:
                k = ('e', o['eng'])
                cnt[k] = cnt.get(k, 0) + 1
                o['tok'] = (k, cnt[k])
            else:
                o['tok'] = None
        sems = {}
        for k in cnt:
            sems[k] = nc.alloc_semaphore("s_%s_%s" % (k[0], str(k[1]).replace(' ', '_')))
        self.sems = sems
        final_tok = {}
        for o in ops:
            if o['dma'] and o['semkey'] in self.final_keys:
                final_tok[o['tok'][0]] = o['tok'][1]
        seen = {e: {} for e in ('pe', 'act', 'dve', 'pool', 'sp')}
        per_eng = {e: [] for e in seen}
        for o in ops:
            e = o['eng']
            waits = []
            for d in o['deps']:
                p = ops[d]
                if (not p['dma']) and (not o['dma']) and p['eng'] == 'pe' and e == 'pe':
                    continue
                k, v = p['tok']
                if p['dma'] and isinstance(p['semkey'], tuple):
                    v = max(q['tok'][1] for q in ops[:ops.index(o)] if q['dma'] and q['semkey'] == p['semkey'])
                if seen[e].get(k, 0) >= v:
                    continue
                seen[e][k] = v
                waits.append((k, v))
            per_eng[e].append((o, waits))

        def run(engname, handle):
            for o, waits in per_eng[engname]:
                for k, v in waits:
                    handle.wait_ge(sems[k], v)
                ins = o['fn'](handle)
                if o['tok'] is not None:
                    k, v = o['tok']
                    ins.then_inc(sems[k], 16 if o['dma'] else 1)
            if engname == 'sp':
                for k, v in final_tok.items():
                    handle.wait_ge(sems[k], v)

        with nc.Block() as block:
            @block.tensor
            def _(t):
                run('pe', t)

            @block.scalar
            def _(s):
                run('act', s)

            @block.vector
            def _(v):
                run('dve', v)

            @block.gpsimd
            def _(g):
                run('pool', g)

            @block.sync
            def _(sy):
                run('sp', sy)
        self.ops = []


S = 2048
D = 1024
NT = 16
PROJ = 4616
EPS = 1e-6
C_QM, C_KM, C_VM, C_OM, C_IF, C_ZM, C_QB, C_KB, C_VB, C_ZB = 0, 512, 1024, 1536, 2048, 2056, 2568, 3080, 3592, 4104
LN_S = float(np.log(128.0 ** -0.5))
NEG = -1.0e30


class Arena:
    def __init__(self, nc, limit=16384 + 207 * 1024):
        self.nc = nc
        self.off = 16384
        self.limit = limit
        self.n = 0

    def alloc(self, name, shape, dtype):
        esz = 4 if dtype == F32 else 2
        per_part = int(np.prod(shape[1:])) * esz
        self.off = (self.off + 63) // 64 * 64
        t = self.nc.alloc_sbuf_tensor_at("%s_%d" % (name, self.n), list(shape), dtype, offset=self.off)
        self.n += 1
        self.off += per_part
        assert self.off <= self.limit, (name, self.off)
        return t

    def mark(self):
        return self.off

    def release(self, m):
        self.off = m


def build_program(upto=99, dumps=()):
    nc = bass.Bass("TRN2", target_bir_lowering=False)
    P = Prog(nc)
    A = Arena(nc)

    def din(name, shape, dt=F32):
        return nc.dram_tensor(name, list(shape), dt, kind="ExternalInput")

    x_d = din("x", [S, D])
    cT_d = din("cT", [128, 8])
    wada_d = din("w_ada", [D, 3 * D])
    bss_d = din("bss", [128, 16])
    bgate_d = din("bgate", [128, D])
    gnT_d = din("gnT", [128, 8])
    win_d = din("w_in", [D, PROJ])
    convw_d = din("convw", [128, 32])
    convb_d = din("convb", [128, 8])
    bif_d = din("bif", [128, 128])
    gml_d = din("gml", [128, 4])
    wout_d = din("w_out", [D, D])
    gfin_d = din("gfin", [128, D])
    identb_d = din("identb", [128, 128], BF16)
    identf_d = din("identf", [128, 128])
    U_d = din("U", [128, 128])
    tri_d = din("tri01", [128, 128], BF16)
    cmask_d = din("cmask", [128, 128], BF16)
    kaug_d = din("kaugc", [11, 8, S], BF16)
    qaug_d = din("qaugc", [3, 8, S], BF16)
    sel_d = din("selc", [8, 8, 64], BF16)
    out_d = nc.dram_tensor("out", [S, D], F32, kind="ExternalOutput")
    scr_d = nc.dram_tensor("scr", [8, 512], F32, kind="Internal")
    dump_d = {}

    PS = nc.alloc_psum_tensor("ps", [128, 8, 512], F32)
    PSB = PS.bitcast(BF16)

    identb = A.alloc("identb", [128, 128], BF16)
    identf = A.alloc("identf", [128, 128], F32)
    U = A.alloc("U", [128, 128], F32)
    onesf = A.alloc("onesf", [128, 128], F32)
    tri01 = A.alloc("tri01", [128, 128], BF16)
    cmask = A.alloc("cmask", [128, 128], BF16)
    gs = A.alloc("gs", [128, 8], F32)
    shiftT = A.alloc("shiftT", [128, 8], F32)
    gate_bc = A.alloc("gate_bc", [128, D], F32)
    convw = A.alloc("convw", [128, 32], F32)
    convb = A.alloc("convb", [128, 8], F32)
    bif = A.alloc("bif", [128, 128], F32)
    gml = A.alloc("gml", [128, 4], F32)
    A.off = (A.off + 63) // 64 * 64
    catT_off = A.off
    catT = A.alloc("catT", [128, 8, S], BF16)
    wbufs = [A.alloc("wbuf%d" % i, [128, 8, 512], BF16) for i in range(2)]
    screp = A.alloc("screp", [128, 8, 128], BF16)
    A.off = (A.off + 63) // 64 * 64
    hT_off = A.off
    hT = A.alloc("hT", [128, 8, S], BF16)
    base_mark = A.mark()

    def ld(dst, src, key, q='sp', group='consts'):
        return P.dma(q, dst, src, writes=[key], group=group)

    win_v = win_d.rearrange("(kc p) c -> p kc c", p=128)
    pre_w = [(wbufs[grp], 'wbuf%d' % grp) for grp in range(2)]

    ld(identb[:, :], identb_d[:, :], 'identb')
    ld(identf[:, :], identf_d[:, :], 'identf')
    ld(U[:, :], U_d[:, :], 'U')
    ld(tri01[:, :], tri_d[:, :], 'tri01')
    ld(cmask[:, :], cmask_d[:, :], 'cmask')
    ld(convw[:, :], convw_d[:, :], 'convw')
    ld(convb[:, :], convb_d[:, :], 'convb')
    ld(bif[:, :], bif_d[:, :], 'bif')
    ld(gml[:, :], gml_d[:, :], 'gml')
    P.op('pool', lambda e: e.memset(onesf[:, :], 1.0), writes=['onesf'])

    def add_dump(name, ap, shape, dt, key):
        d = nc.dram_tensor("dbg_" + name, list(shape), dt, kind="ExternalOutput")
        dump_d[name] = d
        P.dma('sp', d[tuple(slice(None) for _ in shape)], ap, reads=(key if isinstance(key, list) else [key]), writes=['dbg_' + name], final=True)

    m = A.mark()
    xall = A.alloc("xall", [128, NT, D], F32)
    _sv = A.off
    A.off = catT_off
    xn = A.alloc("xn", [128, NT, D], BF16)
    A.off = _sv
    junks = [A.alloc("junk%d" % i, [128, D], BF16) for i in range(2)]
    wst = [A.alloc("wst%d" % i, [128, 2 * D], BF16) for i in range(2)]
    wsf = [A.alloc("wsf%d" % i, [128, 2 * D], F32) for i in range(4)]
    sc = A.alloc("sc", [128, 8], BF16)
    cT = A.alloc("cT", [128, 8], F32)
    ssq = A.alloc("ssq", [128, NT], F32)
    rstd = A.alloc("rstd", [128, NT], F32)
    bss = A.alloc("bss", [128, 16], F32)
    gnT = A.alloc("gnT", [128, 8], F32)
    scl1 = A.alloc("scl1", [128, 8], F32)

    ld(cT[:, :], cT_d[:, :], 'cT')
    ld(bss[:, :], bss_d[:, :], 'bss')
    ld(gnT[:, :], gnT_d[:, :], 'gnT')
    ld(gate_bc[:, :], bgate_d[:, :], 'gate_bc')
    P.op('act', lambda e: e.activation(out=sc[:, :], in_=cT[:, :], func=AF.Silu), reads=['cT'], writes=['sc'])
    for kc in range(8):
        P.op('dve', lambda e, kc=kc: e.tensor_scalar(out=screp[:, kc, :], in0=onesf[:, :], scalar1=sc[:, kc:kc + 1], scalar2=None, op0=ALU.mult),
             reads=['onesf', 'sc'], writes=['screp%d' % kc])
    tcnt = [0]

    def x_group(g, modulate=False):
        for t in range(4 * g, 4 * g + 4):
            P.op('act', lambda e, t=t: e.activation(out=junks[t % 2][:, :], in_=xall[:, t, :], func=AF.Square, accum_out=ssq[:, t:t + 1]),
                 reads=['xall%d' % t], writes=['ssq%d' % t, 'junk%d' % (t % 2)])
        P.op('act', lambda e: e.activation(out=rstd[:, 4 * g:4 * g + 4], in_=ssq[:, 4 * g:4 * g + 4], func=AF.Sqrt, scale=1.0 / D, bias=EPS),
             reads=['ssq%d' % t for t in range(4 * g, 4 * g + 4)], writes=['rstd%d' % g])
        P.op('dve', lambda e: e.reciprocal(rstd[:, 4 * g:4 * g + 4], rstd[:, 4 * g:4 * g + 4]), reads=['rstd%d' % g], writes=['rstd%d' % g])
        for t in range(4 * g, 4 * g + 4):
            P.op('dve', lambda e, t=t: e.tensor_scalar(out=xn[:, t, :], in0=xall[:, t, :], scalar1=rstd[:, t:t + 1], scalar2=None, op0=ALU.mult),
                 reads=['xall%d' % t, 'rstd%d' % g], writes=['xn%d' % t])
        for dc in range(8):
            b = 3 + (tcnt[0] % 4)
            for j in range(4):
                t = 4 * g + j
                P.op('pe', lambda e, b=b, j=j, t=t, dc=dc: e.transpose(PSB[:, b, j * 128:(j + 1) * 128], xn[:, t, dc * 128:(dc + 1) * 128], identb[:, :]),
                     reads=['xn%d' % t, 'identb'], writes=['ps%d' % b])
            if modulate:
                if tcnt[0] % 2 == 0:
                    P.op('act', lambda e, b=b, dc=dc: e.activation(out=hT[:, dc, g * 512:(g + 1) * 512], in_=PSB[:, b, 0:512], func=AF.Identity,
                                                                   scale=gs[:, dc:dc + 1], bias=shiftT[:, dc:dc + 1]),
                         reads=['ps%d' % b, 'gs', 'shiftT'], writes=['hT%d_%d' % (dc, g)])
                else:
                    P.op('dve', lambda e, b=b, dc=dc: e.tensor_scalar(out=hT[:, dc, g * 512:(g + 1) * 512], in0=PSB[:, b, 0:512],
                                                                       scalar1=gs[:, dc:dc + 1], scalar2=shiftT[:, dc:dc + 1], op0=ALU.mult, op1=ALU.add),
                         reads=['ps%d' % b, 'gs', 'shiftT'], writes=['hT%d_%d' % (dc, g)])
            elif tcnt[0] % 2 == 0:
                P.op('act', lambda e, b=b, dc=dc: e.activation(out=hT[:, dc, g * 512:(g + 1) * 512], in_=PSB[:, b, 0:512], func=AF.Copy),
                     reads=['ps%d' % b], writes=['hT%d_%d' % (dc, g)])
            else:
                P.op('dve', lambda e, b=b, dc=dc: e.tensor_copy(hT[:, dc, g * 512:(g + 1) * 512], PSB[:, b, 0:512]),
                     reads=['ps%d' % b], writes=['hT%d_%d' % (dc, g)])
            tcnt[0] += 1

    sfi = 0
    for kc in range(8):
        w = wst[kc % 2]
        wk = 'wst%d' % (kc % 2)
        wf = wsf[sfi % 4]
        wfk = 'wsf%d' % (sfi % 4)
        sfi += 1
        P.dma('pool', wf[:, :], wada_d[kc * 128:(kc + 1) * 128, 0:2048], writes=[wfk])
        P.op('dve', lambda e, w=w, wf=wf: e.tensor_copy(w[:, :], wf[:, :]), reads=[wfk], writes=[wk])
        for t in (2 * kc, 2 * kc + 1):
            last_x_ld = ld(xall[:, t, :], x_d[t * 128:(t + 1) * 128, :], 'xall%d' % t, group='xall%d' % (t // 4))
        for j in range(16):
            P.op('pe', lambda e, w=w, j=j, kc=kc: e.matmul(PS[:, 0, j:j + 1], w[:, j * 128:(j + 1) * 128], sc[:, kc:kc + 1],
                                                            start=(kc == 0 and j == 0), stop=(kc == 7), skip_group_check=True),
                 reads=[wk, 'sc'], writes=['ps0'])
        if kc == 7:
            for grp in range(2):
                P.dma('pool', wbufs[grp][:, :, 0:512], win_v[:, :, C_QM + grp * 512:C_QM + (grp + 1) * 512], writes=['wbuf%d' % grp],
                      extra=[last_x_ld])
        if kc % 2 == 1 and kc >= 3:
            x_group((kc - 3) // 2)
    P.op('dve', lambda e: e.tensor_tensor(out=shiftT[:, :], in0=PS[:, 0, 0:8], in1=bss[:, 0:8], op=ALU.add), reads=['ps0', 'bss'], writes=['shiftT'])
    P.op('dve', lambda e: e.scalar_tensor_tensor(out=scl1[:, :], in0=PS[:, 0, 8:16], scalar=1.0, in1=bss[:, 8:16], op0=ALU.add, op1=ALU.add),
         reads=['ps0', 'bss'], writes=['scl1'])
    P.op('dve', lambda e: e.tensor_tensor(out=gs[:, :], in0=scl1[:, :], in1=gnT[:, :], op=ALU.mult), reads=['scl1', 'gnT'], writes=['gs'])
    x_group(3, modulate=True)
    for dc in range(8):
        eng = 'dve' if dc % 2 == 0 else 'pool'
        P.op(eng, lambda e, dc=dc: e.tensor_scalar(out=hT[:, dc, 0:1536], in0=hT[:, dc, 0:1536], scalar1=gs[:, dc:dc + 1], scalar2=shiftT[:, dc:dc + 1],
                                                   op0=ALU.mult, op1=ALU.add),
             reads=['hT%d_%d' % (dc, g) for g in range(3)] + ['gs', 'shiftT'], writes=['hT%d_%d' % (dc, g) for g in range(3)])
    if 'hT' in dumps:
        add_dump('hT', hT[:, :, :], [128, 8, S], BF16, ['hT%d_%d' % (a, b) for a in range(8) for b in range(4)])
    if 'gate_bc' in dumps:
        add_dump('gate_bc', gate_bc[:, :], [128, D], F32, 'gate_bc')
    A.release(m)
    if upto <= 1:
        P.emit()
        return nc, dump_d

    P.barrier()
    PS4 = PS.reshape([128, 8, 4, 128])
    wslot = [0]

    def load_w(c0, ncols):
        sl = wslot[0] % 2
        wslot[0] += 1
        P.dma('pool', wbufs[sl][:, :, 0:ncols], win_v[:, :, c0:c0 + ncols], writes=['wbuf%d' % sl])
        return wbufs[sl], 'wbuf%d' % sl

    bank_rr = [0]

    def next_bank(lo=0, n=4):
        b = lo + bank_rr[0] % n
        bank_rr[0] += 1
        return b

    evac_rr = [0]

    def evac_copy(out, in_, reads, writes, scale=None, force_act=False):
        evac_rr[0] += 1
        if force_act or evac_rr[0] % 2 == 0:
            if scale is None:
                P.op('act', lambda e: e.activation(out=out, in_=in_, func=AF.Copy), reads=reads, writes=writes)
            else:
                P.op('act', lambda e: e.activation(out=out, in_=in_, func=AF.Copy, scale=scale), reads=reads, writes=writes)
        else:
            if scale is None:
                P.op('dve', lambda e: e.tensor_copy(out, in_), reads=reads, writes=writes)
            else:
                P.op('dve', lambda e: e.tensor_scalar(out=out, in0=in_, scalar1=scale, scalar2=None, op0=ALU.mult), reads=reads, writes=writes)

    def hkeys(g):
        return ['hT%d_%d' % (kc, g) for kc in range(8)]

    def proj_fm(w, wk, cl, g, b):
        for kc in range(8):
            P.op('pe', lambda e, kc=kc: e.matmul(PS[:, b, :], w[:, kc, cl * 128:(cl + 1) * 128], hT[:, kc, g * 512:(g + 1) * 512],
                                                  start=(kc == 0), stop=(kc == 7)),
                 reads=[wk] + hkeys(g), writes=['ps%d' % b])

    def proj_tm(w, wk, t, ncols, out_ap, outkey):
        for kc in range(8):
            P.op('pe', lambda e, kc=kc: e.matmul(out_ap, hT[:, kc, t * 128:(t + 1) * 128], w[:, kc, 0:ncols],
                                                  start=(kc == 0), stop=(kc == 7)),
                 reads=[wk] + hkeys(t // 4), writes=[outkey])

    mB = A.mark()
    qkT = A.alloc("qkT", [128, 8, S], BF16)
    vaug = A.alloc("vaug", [128, NT, 4, 130], BF16)
    A.off = (A.off + 63) // 64 * 64
    gT_off = A.off
    gT = A.alloc("gT", [128, 4, S], BF16)
    _sv2 = A.off
    A.off = gT_off
    wg = A.alloc("wg", [128, 8, D], BF16)
    A.off = _sv2
    u12 = A.alloc("u12", [128, 4, 64], F32)
    thr2 = A.alloc("thr2", [128, 64], F32)
    mB2 = A.mark()
    raw = A.alloc("raw", [128, 8, S + 4], BF16)
    cacc = [A.alloc("cacc%d" % i, [128, 512], F32) for i in range(4)]
    wif = A.alloc("wif", [128, 8, 8], BF16)
    gtmp = [A.alloc("gtmp%d" % i, [128, 512], F32) for i in range(2)]
    szt = [A.alloc("szt%d" % i, [128, 512], BF16) for i in range(2)]
    fi = A.alloc("fi", [128, 128], F32)
    nl = A.alloc("nl", [128, 64], F32)
    linc = A.alloc("linc", [128, 68], F32)
    NF = A.alloc("NF", [128, 64], F32)
    gg = A.alloc("gg", [128, 64], F32)
    tmax = A.alloc("tmax", [128, 1], F32)
    trow = A.alloc("trow", [1, 64], F32)
    Rrow = A.alloc("Rrow", [1, 68], F32)
    Rb = A.alloc("Rb", [128, 68], F32)
    fi3 = fi.reshape([128, 16, 8])
    nl3 = nl.reshape([128, 16, 4])
    NF3 = NF.reshape([128, 16, 4])
    gg3 = gg.reshape([128, 16, 4])

    P.dma('pool', wif[:, :, :], win_v[:, :, C_IF:C_IF + 8], writes=['wif'])
    for k2 in range(8):
        P.dma('pool', wg[:, k2, :], wada_d[k2 * 128:(k2 + 1) * 128, 2048:3072], writes=['wg%d' % k2], group='wg')
    P.op('pool', lambda e: e.memset(raw[:, :, 0:3], 0.0), writes=['rawpad'])
    P.op('pool', lambda e: e.memset(vaug[:, :, :, 128:129], 1.0), writes=['vones'])
    def conv_tile(ct):
        for gp in range(2):
            gs_ = (2 * gp, 2 * gp + 1)
            for g in gs_:
                acc = cacc[g]
                P.op('dve', lambda e, g=g, acc=acc: e.tensor_scalar(out=acc[:, :], in0=raw[:, ct, g * 512 + 3:g * 512 + 3 + 512], scalar1=convw[:, ct * 4 + 3:ct * 4 + 4],
                                                                    scalar2=convb[:, ct:ct + 1], op0=ALU.mult, op1=ALU.add),
                     reads=['raw%d_%d' % (ct, g), 'convw', 'convb'], writes=['cacc%d' % g])
            for tap in range(3):
                for g in gs_:
                    acc = cacc[g]
                    last = (tap == 2)
                    out_ap = qkT[:, ct, g * 512:(g + 1) * 512] if last else acc[:, :]
                    P.op('dve', lambda e, g=g, acc=acc, tap=tap, out_ap=out_ap: e.scalar_tensor_tensor(out=out_ap, in0=raw[:, ct, g * 512 + tap:g * 512 + tap + 512],
                                                                                                       scalar=convw[:, ct * 4 + tap:ct * 4 + tap + 1], in1=acc[:, :],
                                                                                                       op0=ALU.mult, op1=ALU.add),
                         reads=['raw%d_%d' % (ct, g), 'rawpad', 'convw', 'cacc%d' % g] + (['raw%d_%d' % (ct, g - 1)] if g > 0 else []),
                         writes=(['qkTpre%d_%d' % (ct, g)] if last else ['cacc%d' % g]))

    def conv_silu(ct):
        P.op('act', lambda e: e.activation(out=qkT[:, ct, :], in_=qkT[:, ct, :], func=AF.Silu),
             reads=['qkTpre%d_%d' % (ct, g) for g in range(4)], writes=['qkT%d_%d' % (ct, g) for g in range(4)])

    for grp in range(2):
        w, wk = pre_w[grp]
        for cl in range(4):
            ct = grp * 4 + cl
            for g in range(4):
                b = next_bank(0, 4)
                proj_fm(w, wk, cl, g, b)
                evac_copy(raw[:, ct, 3 + g * 512:3 + (g + 1) * 512], PS[:, b, :], ['ps%d' % b], ['raw%d_%d' % (ct, g)], force_act=True)
            if ct >= 1:
                conv_tile(ct - 1)
    w, wk = load_w(C_VM, 512)
    for t in range(NT):
        b = next_bank(0, 4)
        proj_tm(w, wk, t, 512, PS[:, b, :], 'ps%d' % b)
        evac_copy(vaug[:, t, :, 0:128], PS4[:, b, :, :], ['ps%d' % b], ['vaug%d' % t], force_act=True)
    conv_tile(7)
    for t in range(NT):
        proj_tm(wif, 'wif', t, 8, PS[:, 6, t * 8:(t + 1) * 8], 'ps6')
    P.op('dve', lambda e: e.tensor_tensor(out=fi[:, :], in0=PS[:, 6, 0:128], in1=bif[:, :], op=ALU.add), reads=['ps6', 'bif'], writes=['fi'])
    for n in range(2):
        b = next_bank(0, 4)
        for kc in range(8):
            P.op('pe', lambda e, n=n, kc=kc, b=b: e.matmul(PS[:, b, :], screp[:, kc, :], wg[:, kc, n * 512:(n + 1) * 512], start=(kc == 0), stop=(kc == 7)),
                 reads=['wg%d' % kc, 'screp%d' % kc], writes=['ps%d' % b])
        P.op('dve', lambda e, n=n, b=b: e.tensor_tensor(out=gate_bc[:, n * 512:(n + 1) * 512], in0=PS[:, b, :], in1=gate_bc[:, n * 512:(n + 1) * 512], op=ALU.add),
             reads=['ps%d' % b, 'gate_bc'], writes=['gate_bc', 'gate_done%d' % n])
    w_o, wk_o = load_w(C_OM, 512)
    w_z, wk_z = load_w(C_ZM, 512)
    ozg = []
    zi_c = [0]

    def mk_o(cl, g, idx):
        def f():
            if idx % 4 == 1:
                conv_silu(idx // 4)
            b = next_bank(0, 4)
            proj_fm(w_o, wk_o, cl, g, b)
            P.op('act', lambda e: e.activation(out=gT[:, cl, g * 512:(g + 1) * 512], in_=PS[:, b, :], func=AF.Sigmoid),
                 reads=['ps%d' % b, 'gate_done0', 'gate_done1'], writes=['gT%d_%d' % (cl, g)])
        return f

    def mk_z(cl, g, idx):
        def f():
            if idx % 4 == 1:
                conv_silu(4 + idx // 4)
            b = next_bank(0, 4)
            proj_fm(w_z, wk_z, cl, g, b)
            sz = szt[zi_c[0] % 2]
            szk = 'szt%d' % (zi_c[0] % 2)
            zi_c[0] += 1
            P.op('act', lambda e: e.activation(out=sz[:, :], in_=PS[:, b, :], func=AF.Silu), reads=['ps%d' % b], writes=[szk])
            P.op('dve', lambda e: e.scalar_tensor_tensor(out=gT[:, cl, g * 512:(g + 1) * 512], in0=gT[:, cl, g * 512:(g + 1) * 512],
                                                         scalar=gml[:, cl:cl + 1], in1=sz[:, :], op0=ALU.mult, op1=ALU.mult),
                 reads=['gT%d_%d' % (cl, g), szk, 'gml'], writes=['gT%d_%d' % (cl, g)])
        return f

    for cl in range(4):
        for g in range(4):
            ozg.append(mk_o(cl, g, cl * 4 + g))
    for cl in range(4):
        for g in range(4):
            ozg.append(mk_z(cl, g, cl * 4 + g))

    def emit_oz(n):
        for _ in range(n):
            if ozg:
                ozg.pop(0)()

    P.op('act', lambda e: e.activation(out=nl3[:, :, :], in_=fi3[:, :, 4:8], func=AF.Exp, scale=-1.0), reads=['fi'], writes=['nl'])
    P.op('act', lambda e: e.activation(out=nl[:, :], in_=nl[:, :], func=AF.Ln, bias=1.0), reads=['nl'], writes=['nl'])
    P.op('dve', lambda e: e.memset(linc[:, 0:4], 0.0), writes=['linc'])
    for t in range(16):
        P.op('dve', lambda e, t=t: e.tensor_tensor(out=linc[:, (t + 1) * 4:(t + 2) * 4], in0=linc[:, t * 4:(t + 1) * 4], in1=nl[:, t * 4:(t + 1) * 4], op=ALU.add),
             reads=['linc', 'nl'], writes=['linc'])
    emit_oz(5)
    P.op('pe', lambda e: e.matmul(PS[:, 7, 0:64], U[:, :], nl[:, :], start=True, stop=False), reads=['U', 'nl'], writes=['ps7'])
    P.op('pe', lambda e: e.matmul(PS[:, 7, 0:64], onesf[:, :], linc[:, 0:64], start=False, stop=True), reads=['onesf', 'linc'], writes=['ps7'])
    P.op('dve', lambda e: e.tensor_copy(NF[:, :], PS[:, 7, 0:64]), reads=['ps7'], writes=['NF'])
    P.op('dve', lambda e: e.tensor_tensor(out=gg3[:, :, :], in0=fi3[:, :, 0:4], in1=NF3[:, :, :], op=ALU.add), reads=['fi', 'NF'], writes=['gg'])
    emit_oz(3)
    P.op('pe', lambda e: e.transpose(PS[0:64, 7, 128:256], gg[:, 0:64], identf[:, :]), reads=['gg', 'identf'], writes=['ps7b'])
    P.op('dve', lambda e: e.tensor_reduce(out=tmax[0:64, 0:1], in_=PS[0:64, 7, 128:256], op=ALU.max, axis=mybir.AxisListType.X), reads=['ps7b'], writes=['tmax'])
    emit_oz(3)
    P.op('pe', lambda e: e.transpose(PS[0:1, 7, 256:320], tmax[0:64, 0:1], identf[0:64, 0:64]), reads=['tmax', 'identf'], writes=['ps7c'])
    P.op('dve', lambda e: e.tensor_copy(trow[0:1, :], PS[0:1, 7, 256:320]), reads=['ps7c'], writes=['trow'])
    P.op('dve', lambda e: e.memset(Rrow[0:1, 0:4], 0.0), writes=['Rrow'])
    for t in range(16):
        P.op('dve', lambda e, t=t: e.tensor_tensor(out=Rrow[0:1, (t + 1) * 4:(t + 2) * 4], in0=Rrow[0:1, t * 4:(t + 1) * 4], in1=trow[0:1, t * 4:(t + 1) * 4], op=ALU.max),
             reads=['Rrow', 'trow'], writes=['Rrow'])
    emit_oz(6)
    P.op('pe', lambda e: e.matmul(PS[:, 7, 320:388], onesf[0:1, 0:128], Rrow[0:1, 0:68], start=True, stop=True), reads=['onesf', 'Rrow'], writes=['ps7d'])
    emit_oz(4)
    P.op('dve', lambda e: e.tensor_copy(Rb[:, :], PS[:, 7, 320:388]), reads=['ps7d'], writes=['Rb'])
    P.op('dve', lambda e: e.tensor_tensor(out=u12[:, 0, :], in0=gg[:, :], in1=Rb[:, 0:64], op=ALU.subtract), reads=['gg', 'Rb'], writes=['u12'])
    P.op('dve', lambda e: e.tensor_tensor(out=u12[:, 1, :], in0=gg[:, :], in1=Rb[:, 4:68], op=ALU.subtract), reads=['gg', 'Rb'], writes=['u12'])
    P.op('dve', lambda e: e.tensor_tensor(out=u12[:, 2, :], in0=NF[:, :], in1=Rb[:, 0:64], op=ALU.subtract), reads=['NF', 'Rb'], writes=['u12'])
    P.op('dve', lambda e: e.tensor_tensor(out=u12[:, 3, :], in0=Rb[:, 0:64], in1=Rb[:, 4:68], op=ALU.subtract), reads=['Rb'], writes=['u12'])
    P.op('act', lambda e: e.activation(out=u12[:, 0:2, :], in_=u12[:, 0:2, :], func=AF.Exp, bias=LN_S), reads=['u12'], writes=['u12'])
    P.op('act', lambda e: e.activation(out=u12[:, 2:4, :], in_=u12[:, 2:4, :], func=AF.Exp), reads=['u12'], writes=['u12'])
    P.op('pool', lambda e: e.tensor_tensor(out=thr2[:, :], in0=u12[:, 2, :], in1=u12[:, 2, :], op=ALU.mult), reads=['u12'], writes=['thr2'])
    emit_oz(len(ozg))
    preC = [load_w(C_QB, 512), load_w(C_KB, 512)]
    if 'qkT' in dumps:
        add_dump('qkT', qkT[:, :, :], [128, 8, S], BF16, ['qkT%d_%d' % (a, b) for a in range(8) for b in range(4)])
    if 'u12' in dumps:
        add_dump('u12', u12[:, :, :], [128, 4, 64], F32, 'u12')
    if 'gT' in dumps:
        add_dump('gT', gT[:, :, :], [128, 4, S], BF16, ['gT%d_%d' % (a, b) for a in range(4) for b in range(4)])
    A.release(mB2)
    if upto <= 2:
        P.emit()
        return nc, dump_d

    P.barrier(exclude=('wbuf0', 'wbuf1'))
    stil = [A.alloc("stil%d" % i, [128, 128], BF16) for i in range(2)]
    ktil = [A.alloc("ktil%d" % i, [128, 128], BF16) for i in range(2)]
    Cf = [A.alloc("Cf%d" % i, [128, 130], F32) for i in range(4)]
    Cb = [A.alloc("Cb%d" % i, [128, 130], BF16) for i in range(4)]
    Asb = A.alloc("Asb", [128, NT, 4, 130], F32)
    Bsb = A.alloc("Bsb", [128, 64], F32)
    ssqA = A.alloc("ssqA", [128, 64], F32)
    junkA = [A.alloc("junkA%d" % i, [128, 128], BF16) for i in range(2)]
    Af = [A.alloc("Af%d" % i, [128, 130], F32) for i in range(2)]
    sm = A.alloc("sm", [128, 4, 64], F32)
    hnb = [A.alloc("hnb%d" % i, [128, 128], BF16) for i in range(4)]
    stepsB = [(t, h) for t in range(NT) for h in range(4)]

    def stage1(i):
        t, h = stepsB[i]
        par = i % 2
        col = t * 4 + h
        tl = slice(t * 128, (t + 1) * 128)
        qk_q = 'qkT%d_%d' % (h, t // 4)
        qk_k = 'qkT%d_%d' % (4 + h, t // 4)
        P.op('pe', lambda e: e.matmul(PS[:, par, 0:128], qkT[:, 4 + h, tl], qkT[:, h, tl], start=True, stop=True),
             reads=[qk_q, qk_k], writes=['ps%d' % par])
        P.op('dve', lambda e: e.scalar_tensor_tensor(out=stil[par][:, :], in0=PS[:, par, 0:128], scalar=u12[:, 0, col:col + 1], in1=tri01[:, :],
                                                     op0=ALU.mult, op1=ALU.mult),
             reads=['ps%d' % par, 'u12', 'tri01'], writes=['stil%d' % par])
        if t < NT - 1:
            P.op('pe', lambda e: e.transpose(PSB[:, 4 + par, 0:128], qkT[:, 4 + h, tl], identb[:, :]),
                 reads=[qk_k, 'identb'], writes=['ps%d' % (4 + par)])
            P.op('act', lambda e: e.activation(out=ktil[par][:, :], in_=PSB[:, 4 + par, 0:128], func=AF.Copy, scale=u12[:, 1, col:col + 1]),
                 reads=['ps%d' % (4 + par), 'u12'], writes=['ktil%d' % par])

    def stage2(i):
        t, h = stepsB[i]
        par = i % 2
        col = t * 4 + h
        tl = slice(t * 128, (t + 1) * 128)
        qk_q = 'qkT%d_%d' % (h, t // 4)
        P.op('pe', lambda e: e.matmul(PS[:, 2 + par, 0:129], stil[par][:, :], vaug[:, t, h, 0:129], start=True, stop=(t == 0)),
             reads=['stil%d' % par, 'vaug%d' % t, 'vones'], writes=['ps%d' % (2 + par)])
        if t > 0:
            P.op('pe', lambda e: e.matmul(PS[:, 2 + par, 0:129], qkT[:, h, tl], Cb[h][:, 0:129], start=False, stop=True),
                 reads=[qk_q, 'Cb%d' % h], writes=['ps%d' % (2 + par)])
        if t < NT - 1:
            P.op('pe', lambda e: e.matmul(PS[:, 6 + par, 0:129], ktil[par][:, :], vaug[:, t, h, 0:129], start=True, stop=True),
                 reads=['ktil%d' % par, 'vaug%d' % t, 'vones'], writes=['ps%d' % (6 + par)])
            if t == 0:
                P.op('dve', lambda e: e.tensor_copy(Cf[h][:, 0:129], PS[:, 6 + par, 0:129]), reads=['ps%d' % (6 + par)], writes=['Cf%d' % h])
            else:
                P.op('dve', lambda e: e.scalar_tensor_tensor(out=Cf[h][:, 0:129], in0=Cf[h][:, 0:129], scalar=u12[:, 3, col:col + 1],
                                                             in1=PS[:, 6 + par, 0:129], op0=ALU.mult, op1=ALU.add),
                     reads=['ps%d' % (6 + par), 'u12', 'Cf%d' % h], writes=['Cf%d' % h])
            P.op('pool', lambda e: e.tensor_copy(Cb[h][:, 0:129], Cf[h][:, 0:129]), reads=['Cf%d' % h], writes=['Cb%d' % h])
        P.op('dve', lambda e: e.tensor_copy(Asb[:, t, h, 0:129], PS[:, 2 + par, 0:129]), reads=['ps%d' % (2 + par)], writes=['Asb%d_%d' % (t, h)])
        P.op('act', lambda e: e.activation(out=junkA[par][:, :], in_=Asb[:, t, h, 0:128], func=AF.Square, accum_out=ssqA[:, col:col + 1]),
             reads=['Asb%d_%d' % (t, h)], writes=['ssqA%d' % col, 'junkA%d' % par])

    for i in range(len(stepsB) + 1):
        if i < len(stepsB):
            stage1(i)
        if i >= 1:
            stage2(i - 1)
    allB = ['Asb%d_%d' % (t_, h_) for t_ in range(NT) for h_ in range(4)]
    allS = ['ssqA%d' % c for c in range(64)]
    Bsb3 = Bsb.reshape([128, NT, 4])
    P.op('dve', lambda e: e.tensor_copy(Bsb3[:, :, :], Asb[:, :, :, 128]), reads=allB, writes=['Bsb'])
    P.op('dve', lambda e: e.tensor_tensor(out=sm[:, 0, :], in0=Bsb[:, :], in1=Bsb[:, :], op=ALU.mult), reads=['Bsb'], writes=['sm0'])
    P.op('dve', lambda e: e.tensor_tensor(out=sm[:, 0, :], in0=sm[:, 0, :], in1=thr2[:, :], op=ALU.max), reads=['thr2', 'sm0'], writes=['sm0'])
    P.op('dve', lambda e: e.scalar_tensor_tensor(out=sm[:, 2, :], in0=sm[:, 0, :], scalar=128.0 * EPS, in1=ssqA[:, :], op0=ALU.mult, op1=ALU.add),
         reads=['sm0'] + allS, writes=['sm2'])
    P.op('act', lambda e: e.activation(out=sm[:, 2, :], in_=sm[:, 2, :], func=AF.Sqrt, scale=1.0 / 128.0), reads=['sm2'], writes=['sm2'])
    P.op('dve', lambda e: e.reciprocal(sm[:, 3, :], sm[:, 2, :]), reads=['sm2'], writes=['sm3'])
    cnt = 0
    for g in range(4):
        for h in range(4):
            b = cnt % 2
            cnt += 1
            for j in range(4):
                t = 4 * g + j
                col = t * 4 + h
                sl = (cnt * 4 + j) % 4
                if j == 3:
                    P.op('dve', lambda e, sl=sl, t=t, h=h, col=col: e.tensor_scalar(out=hnb[sl][:, :], in0=Asb[:, t, h, 0:128], scalar1=sm[:, 3, col:col + 1], scalar2=None, op0=ALU.mult),
                         reads=['Asb%d_%d' % (t, h), 'sm3'], writes=['hnb%d' % sl])
                elif j % 2 == 0:
                    P.op('act', lambda e, sl=sl, t=t, h=h, col=col: e.activation(out=hnb[sl][:, :], in_=Asb[:, t, h, 0:128], func=AF.Copy, scale=sm[:, 3, col:col + 1]),
                         reads=['Asb%d_%d' % (t, h), 'sm3'], writes=['hnb%d' % sl])
                else:
                    P.op('pool', lambda e, sl=sl, t=t, h=h, col=col: e.tensor_scalar(out=hnb[sl][:, :], in0=Asb[:, t, h, 0:128], scalar1=sm[:, 3, col:col + 1], scalar2=1.0,
                                                                                     op0=ALU.mult, op1=ALU.mult),
                         reads=['Asb%d_%d' % (t, h), 'sm3'], writes=['hnb%d' % sl])
                P.op('pe', lambda e, sl=sl, b=b, j=j: e.transpose(PSB[:, b, j * 128:(j + 1) * 128], hnb[sl][:, :], identb[:, :]),
                     reads=['hnb%d' % sl, 'identb'], writes=['ps%d' % b])
            P.op('dve', lambda e, b=b, g=g, h=h: e.tensor_tensor(out=catT[:, h, g * 512:(g + 1) * 512], in0=PSB[:, b, 0:512], in1=gT[:, h, g * 512:(g + 1) * 512], op=ALU.mult),
                 reads=['ps%d' % b, 'gT%d_%d' % (h, g)], writes=['catT%d_%d' % (h, g)])
    if 'catA' in dumps:
        add_dump('catA', catT[:, 0:4, :], [128, 4, S], BF16, ['catT%d_%d' % (a, b) for a in range(4) for b in range(4)])
    if 'Asb' in dumps:
        add_dump('Asb', Asb[:, :, :, :], [128, NT, 4, 128], BF16, ['Asb%d_%d' % (a, b) for a in range(NT) for b in range(4)])
    if 'sm' in dumps:
        add_dump('sm', sm[:, :, :], [128, 4, 64], F32, ['sm0', 'sm1', 'sm2', 'sm3'])
    A.release(mB)
    if upto <= 3:
        P.emit()
        return nc, dump_d

    P.barrier()
    mC = A.mark()
    PS8 = PS.reshape([128, 8, 8, 64])
    PSg = PS.reshape([128, 8, 64, 8])
    A.off = (A.off + 63) // 64 * 64
    Qaug_off = A.off
    Qaug = A.alloc("Qaug", [128, 8, S], BF16)
    Kaug = A.alloc("Kaug", [128, 8, S], BF16)
    Kaug4 = Kaug.reshape([128, 8, 8, 256])
    vb = A.alloc("vb", [128, NT, 8, 72], BF16)
    vb_end = A.off
    szb = A.alloc("szb", [128, 4, S], BF16)
    A.off = (A.off + 63) // 64 * 64
    qst_off = A.off
    qst = [A.alloc("qst%d" % i, [128, S], BF16) for i in range(2)]
    sel = A.alloc("sel", [128, 8, 64], BF16)
    kmf = A.alloc("kmf", [128, 64], F32)
    kmb = A.alloc("kmb", [128, 8, 8], BF16)
    gsb = A.alloc("gsb", [128, 8, 8, 8], F32)
    t8 = A.alloc("t8", [128, 8, 8, 8], F32)
    mq = A.alloc("mq", [128, 8, 8, 8], BF16)
    mT = A.alloc("mT", [128, 1024], BF16)
    kmb2 = kmb.reshape([128, 64])

    P.dma('sp', Kaug[64:75, :, :], kaug_d[:, :, :], writes=['Kconst'], group='cconst')
    P.dma('sp', Qaug[72:75, :, :], qaug_d[:, :, :], writes=['Qconst'], group='cconst')
    P.dma('sp', sel[64:72, :, :], sel_d[:, :, :], writes=['sel'], group='cconst')
    P.op('pool', lambda e: e.memset(Qaug[64:72, :, 0:1024], 0.0), writes=['Qmzero'])
    P.op('pool', lambda e: e.memset(vb[:, :, :, 64:72], 0.0), writes=['vbones'])
    for h in range(8):
        P.op('pool', lambda e, h=h: e.memset(vb[:, :, h, 64 + h:65 + h], 1.0), reads=['vbones'], writes=['vbones'])
    P.op('pool', lambda e: e.memset(gsb[:, :, :, :], NEG), writes=['gsbinit'])
    sti = 0
    for which, c0, dst, nm, scl in (('q', C_QB, Qaug, 'Qd', 0.125), ('k', C_KB, Kaug, 'Kd', None)):
        w, wk = preC[0 if which == 'q' else 1]
        for cl in range(4):
            st = qst[sti % 2]
            stk = 'qst%d' % (sti % 2)
            sti += 1
            for g in range(4):
                b = next_bank(0, 4)
                proj_fm(w, wk, cl, g, b)
                evac_copy(st[:, g * 512:(g + 1) * 512], PS[:, b, :], ['ps%d' % b], [stk + '_%d' % g], scale=scl)
            sk = [stk + '_%d' % g for g in range(4)]
            P.dma('sp', dst[0:64, 2 * cl, :], st[0:64, :], reads=sk, writes=['%s%d' % (nm, 2 * cl)], group=nm)
            P.dma('sp', dst[0:64, 2 * cl + 1, :], st[64:128, :], reads=sk, writes=['%s%d' % (nm, 2 * cl + 1)], group=nm)
    for h in range(8):
        P.op('dve', lambda e, h=h: e.tensor_reduce(out=kmf[0:64, h * 8:(h + 1) * 8], in_=Kaug4[0:64, h, :, :], op=ALU.add, axis=mybir.AxisListType.X),
             reads=['Kd%d' % h], writes=['kmf%d' % h])
    P.op('dve', lambda e: e.tensor_scalar(out=kmb2[0:64, :], in0=kmf[0:64, :], scalar1=1.0 / 256.0, scalar2=None, op0=ALU.mult),
         reads=['kmf%d' % h for h in range(8)], writes=['kmb'])
    w, wk = load_w(C_VB, 512)
    for t in range(NT):
        b = next_bank(0, 4)
        proj_tm(w, wk, t, 512, PS[:, b, :], 'ps%d' % b)
        evac_copy(vb[:, t, :, 0:64], PS8[:, b, :, :], ['ps%d' % b], ['vb%d' % t], force_act=True)
    def gate_tile(qt):
        j = qt // 2
        i2 = qt % 2
        gb = 4 + i2
        for h in range(8):
            P.op('pe', lambda e, h=h: e.matmul(PS[:, gb, h * 8:(h + 1) * 8], Qaug[0:64, h, qt * 128:(qt + 1) * 128], kmb[0:64, h, :], start=True, stop=True),
                 reads=['Qd%d' % h, 'kmb'], writes=['ps%d' % gb])
        P.op('dve', lambda e: e.tensor_copy(gsb[:, qt - 8, :, 0:j], PSg[:, gb, 0:8, 0:j]), reads=['ps%d' % gb, 'gsbinit'], writes=['gsb%d' % qt])
        for h in range(8):
            P.op('dve', lambda e, h=h: e.max(t8[:, qt - 8, h, :], gsb[:, qt - 8, h, :]), reads=['gsb%d' % qt], writes=['t8_%d_%d' % (qt, h)])
            P.op('dve', lambda e, h=h: e.tensor_scalar(out=mq[:, qt - 8, h, :], in0=gsb[:, qt - 8, h, :], scalar1=t8[:, qt - 8, h, 2:3], scalar2=1.0,
                                                       op0=ALU.is_ge, op1=ALU.subtract),
                 reads=['gsb%d' % qt, 't8_%d_%d' % (qt, h)], writes=['mq%d' % qt])
        P.op('dve', lambda e: e.memset(mq[:, qt - 8, :, j:j + 1], 0.0), reads=['mq%d' % qt], writes=['mq%d' % qt])

    w, wk = load_w(C_ZB, 512)
    zi_ = 0
    for cl in range(4):
        for g in range(4):
            if zi_ % 2 == 0:
                gate_tile(8 + zi_ // 2)
            zi_ += 1
            b = next_bank(0, 4)
            proj_fm(w, wk, cl, g, b)
            P.op('act', lambda e, b=b, cl=cl, g=g: e.activation(out=szb[:, cl, g * 512:(g + 1) * 512], in_=PS[:, b, :], func=AF.Silu),
                 reads=['ps%d' % b], writes=['szb%d_%d' % (cl, g)])
    mqf = mq.reshape([128, 8, 64])
    qm_ops = []

    def mask_finish():
        for qt in range(8, 16):
            P.op('pe', lambda e, qt=qt: e.transpose(PSB[0:64, 7, (qt - 8) * 128:(qt - 7) * 128], mqf[:, qt - 8, :], identb[:, :]),
                 reads=['mq%d' % qt, 'identb'], writes=['ps7'])
        P.op('dve', lambda e: e.tensor_copy(mT[0:64, :], PSB[0:64, 7, 0:1024]), reads=['ps7'], writes=['mT'])
        for h in range(8):
            qm_ops.append(P.dma('sp', Qaug[64:72, h, 1024:2048], mT[8 * h:8 * h + 8, :], reads=['mT'], writes=['Qm%d' % h], group='Qm'))

    wout_v = wout_d.rearrange("(kc p) c -> p kc c", p=128)
    for n in range(2):
        P.dma('pool', wbufs[n][:, :, :], wout_v[:, :, n * 512:(n + 1) * 512], writes=['wbuf%d' % n])
    for n in range(2):
        for kc in range(8):
            P.op('pool', lambda e, n=n, kc=kc: e.tensor_tensor(out=wbufs[n][:, kc, :], in0=wbufs[n][:, kc, :], in1=gate_bc[:, n * 512:(n + 1) * 512], op=ALU.mult),
                 reads=['wbuf%d' % n, 'gate_bc'], writes=['wbuf%d' % n])
    cur = A.off
    A.off = hT_off
    pt = [A.alloc("pt%d" % i, [128, 2, 512], BF16) for i in range(3)]
    onum2 = [A.alloc("onum%d" % i, [128, 8, 512], BF16) for i in range(2)]
    dens = A.alloc("dens", [128, 512], F32)
    rdf = A.alloc("rdf", [128, 512], F32)
    ost = [A.alloc("ost%d" % i, [128, 512], BF16) for i in range(4)]
    assert A.off <= hT_off + 8 * S * 2
    assert cur - qst_off >= 16384, (cur, qst_off)
    A.off = qst_off
    bc = A.alloc("bc", [128, 8, 512], F32)
    A.off = cur
    stepsC = []
    for qc in range(4):
        for h in range(8):
            lst = [('pair', pr) for pr in range(2 * qc)] + [('diag', r) for r in range(4)]
            for n_, (kind, idx) in enumerate(lst):
                stepsC.append((qc, h, kind, idx, n_ == 0, n_ == len(lst) - 1))
    osti_ = [0]
    defer = []

    def tick(flush=False):
        for d in defer:
            d[0] -= 1
        while defer and (flush or defer[0][0] <= 0):
            defer.pop(0)[1]()

    def szb_mult(c, g):
        ck = 'catT%d_%d' % (4 + c, g)
        P.op('dve', lambda e: e.tensor_tensor(out=catT[:, 4 + c, g * 512:(g + 1) * 512], in0=catT[:, 4 + c, g * 512:(g + 1) * 512],
                                              in1=szb[:, c, g * 512:(g + 1) * 512], op=ALU.mult),
             reads=[ck + 'a', ck + 'b', 'szb%d_%d' % (c, g)], writes=[ck])

    def att_front(i):
        qc, h, kind, idx, isfirst, islast = stepsC[i]
        st_ = i % 3
        qs = slice(qc * 512, (qc + 1) * 512)
        rk = ['Qd%d' % h, 'Qconst', 'Qmzero', 'Kd%d' % h, 'Kconst'] + (['Qm%d' % h] if qc >= 2 else [])
        if kind == 'pair':
            for i2 in range(2):
                kt = 2 * idx + i2
                P.op('pe', lambda e, i2=i2, kt=kt: e.matmul(PS[:, 2 * st_ + i2, :], Kaug[0:75, h, kt * 128:(kt + 1) * 128], Qaug[0:75, h, qs], start=True, stop=True),
                     reads=rk, writes=['ps%d' % (2 * st_ + i2)])
            for i2 in range(2):
                P.op('act', lambda e, i2=i2: e.activation(out=pt[st_][:, i2, :], in_=PS[:, 2 * st_ + i2, :], func=AF.Exp),
                     reads=['ps%d' % (2 * st_ + i2)], writes=['pt%d_%d' % (st_, i2)])
        else:
            r = idx
            kt = 4 * qc + r
            c0 = 128 * r
            bk = 2 * st_
            P.op('pe', lambda e: e.matmul(PS[:, bk, c0:512], Kaug[0:75, h, kt * 128:(kt + 1) * 128], Qaug[0:75, h, qc * 512 + c0:(qc + 1) * 512],
                                          start=True, stop=False, skip_group_check=True),
                 reads=rk, writes=['ps%d' % bk])
            P.op('pe', lambda e: e.matmul(PS[:, bk, c0:c0 + 128], identb[:, :], cmask[:, :], start=False, stop=True, skip_group_check=True),
                 reads=['identb', 'cmask'], writes=['ps%d' % bk])
            P.op('act', lambda e: e.activation(out=pt[st_][:, 0, c0:512], in_=PS[:, bk, c0:512], func=AF.Exp),
                 reads=['ps%d' % bk], writes=['pt%d_0' % st_])

    def att_back(i):
        qc, h, kind, idx, isfirst, islast = stepsC[i]
        st_ = i % 3
        qs = slice(qc * 512, (qc + 1) * 512)
        ob = 6 + h % 2
        okey = 'ps%d' % ob
        if kind == 'pair':
            for i2 in range(2):
                kt = 2 * idx + i2
                P.op('pe', lambda e, i2=i2, kt=kt: e.matmul(PS[0:72, ob, :], vb[:, kt, h, 0:72], pt[st_][:, i2, :],
                                                            start=(isfirst and i2 == 0), stop=False, skip_group_check=True),
                     reads=['pt%d_%d' % (st_, i2), 'vb%d' % kt, 'vbones'], writes=[okey])
        else:
            r = idx
            kt = 4 * qc + r
            c0 = 128 * r
            P.op('pe', lambda e: e.matmul(PS[0:72, ob, c0:512], vb[:, kt, h, 0:72], pt[st_][:, 0, c0:512],
                                          start=isfirst, stop=(r == 3), skip_group_check=True),
                 reads=['pt%d_0' % st_, 'vb%d' % kt, 'vbones'], writes=[okey])
        if not islast:
            return
        onum = onum2[qc % 2]
        onk = 'onum%d_' % (qc % 2)
        P.op('dve', lambda e: e.tensor_copy(onum[0:64, h, :], PS[0:64, ob, :]), reads=[okey], writes=[onk + '%d' % h])
        if h == 0:
            P.op('dve', lambda e: e.tensor_copy(dens[64:72, :], PS[64:72, ob, :]), reads=[okey], writes=['dens'])
        else:
            P.op('dve', lambda e: e.tensor_tensor(out=dens[64:72, :], in0=dens[64:72, :], in1=PS[64:72, ob, :], op=ALU.add), reads=[okey, 'dens'], writes=['dens'])
        tick()
        if h % 4 != 3:
            return
        gi = h // 4
        hi_ = 64 + 4 * (gi + 1)
        P.op('dve', lambda e: e.reciprocal(rdf[64:hi_, :], dens[64:hi_, :]), reads=['dens'], writes=['rdf'])
        P.dma('sp', scr_d[4 * gi:4 * gi + 4, :], rdf[64 + 4 * gi:68 + 4 * gi, :], reads=['rdf'], writes=['scr%d' % gi], extra=list(qm_ops))
        for hh in range(4 * gi, 4 * gi + 4):
            P.dma('sp', bc[0:64, hh:hh + 1, :], scr_d[hh:hh + 1, :].partition_broadcast(64), reads=['scr%d' % gi], writes=['bc%d' % hh], extra=list(qm_ops))

        def part2(qc=qc, gi=gi, qs=qs, onum=onum, onk=onk):
            for hh in range(4 * gi, 4 * gi + 4):
                ck = 'catT%d_%d' % (4 + hh // 2, qc)
                if hh % 2 == 0:
                    P.op('dve', lambda e, hh=hh: e.tensor_tensor(out=catT[0:64, 4 + hh // 2, qs], in0=onum[0:64, hh, :], in1=bc[0:64, hh, :], op=ALU.mult),
                         reads=['bc%d' % hh, onk + '%d' % hh], writes=[ck + 'a'])
                else:
                    o_ = ost[osti_[0] % 4]
                    ok_ = 'ost%d' % (osti_[0] % 4)
                    osti_[0] += 1
                    P.op('dve', lambda e, hh=hh, o_=o_: e.tensor_tensor(out=o_[0:64, :], in0=onum[0:64, hh, :], in1=bc[0:64, hh, :], op=ALU.mult),
                         reads=['bc%d' % hh, onk + '%d' % hh], writes=[ok_])
                    P.dma('sp', catT[64:128, 4 + hh // 2, qs], o_[0:64, :], reads=[ok_], writes=[ck + 'b'], group='catTb%d_%d' % (qc, gi))

        def part3(qc=qc, gi=gi):
            szb_mult(2 * gi, qc)
            szb_mult(2 * gi + 1, qc)

        defer.append([2, part2])
        defer.append([4, part3])

    for i in range(len(stepsC) + 1):
        if i < len(stepsC):
            att_front(i)
        if i >= 1:
            att_back(i - 1)
        if i == 10:
            mask_finish()
    assert len(qm_ops) == 8
    last_att_pe = max(i for i, o in enumerate(P.ops) if o['eng'] == 'pe' and not o['dma'])
    if 'catB' in dumps:
        add_dump('catB', catT[:, 4:8, :], [128, 4, S], BF16, ['catT%d_%d' % (a, b) for a in range(4, 8) for b in range(4)])
    A.release(mC)
    if upto <= 4:
        P.emit()
        return nc, dump_d

    P.fence([last_att_pe])
    A.off = Qaug_off
    gfin = A.alloc("gfin", [128, D], F32)
    xr = [A.alloc("xr%d" % i, [128, D], F32) for i in range(4)]
    pre = A.alloc("pre", [128, 8, D], F32)
    ot = [A.alloc("ot%d" % i, [128, D], F32) for i in range(6)]
    ssqo = A.alloc("ssqo", [128, NT], F32)
    rso = A.alloc("rso", [128, NT], F32)
    jk = [A.alloc("jk%d" % i, [128, D], BF16) for i in range(2)]
    assert A.off <= vb_end, (A.off, vb_end)
    ld(gfin[:, :], gfin_d[:, :], 'gfin', group='gfin')

    def fin_tile(tt):
        g4 = tt // 4
        ok_ = 'ot%d' % (tt % 6)
        P.op('dve', lambda e: e.scalar_tensor_tensor(out=ot[tt % 6][:, :], in0=pre[:, tt % 8, :], scalar=rso[:, tt:tt + 1], in1=gfin[:, :], op0=ALU.mult, op1=ALU.mult),
             reads=['pre%d_0' % (tt % 8), 'pre%d_1' % (tt % 8), 'rso%d' % g4, 'gfin'], writes=[ok_])
        P.dma('pool', out_d[tt * 128:(tt + 1) * 128, :], ot[tt % 6][:, :], reads=[ok_], writes=['out'], final=True)

    pend = []

    def x_reload(t):
        P.dma('act', xr[t % 4][:, :], x_d[t * 128:(t + 1) * 128, :], writes=['xr%d' % (t % 4)])

    for t in range(3):
        x_reload(t)
    for t in range(NT):
        xk = 'xr%d' % (t % 4)
        if t + 3 < NT:
            x_reload(t + 3)
        for n in range(2):
            b = next_bank(0, 4)
            for kc in range(8):
                P.op('pe', lambda e, b=b, kc=kc, t=t, n=n: e.matmul(PS[:, b, :], catT[:, kc, t * 128:(t + 1) * 128], wbufs[n][:, kc, :],
                                                                   start=(kc == 0), stop=(kc == 7)),
                     reads=['wbuf%d' % n, 'catT%d_%d' % (kc, t // 4)], writes=['ps%d' % b])
            P.op('dve', lambda e, b=b, t=t, n=n: e.tensor_tensor(out=pre[:, t % 8, n * 512:(n + 1) * 512], in0=PS[:, b, :], in1=xr[t % 4][:, n * 512:(n + 1) * 512], op=ALU.add),
                 reads=['ps%d' % b, xk], writes=['pre%d_%d' % (t % 8, n)])
        P.op('act', lambda e, t=t: e.activation(out=jk[t % 2][:, :], in_=pre[:, t % 8, :], func=AF.Square, accum_out=ssqo[:, t:t + 1]),
             reads=['pre%d_0' % (t % 8), 'pre%d_1' % (t % 8)], writes=['ssqo%d' % t, 'jk%d' % (t % 2)])
        if pend:
            fin_tile(pend.pop(0))
        if t == 2 or t == 5:
            tick()
            tick()
        if t == 8:
            tick(flush=True)
        if t % 4 == 3:
            g4 = t // 4
            P.op('act', lambda e, g4=g4: e.activation(out=rso[:, g4 * 4:(g4 + 1) * 4], in_=ssqo[:, g4 * 4:(g4 + 1) * 4], func=AF.Sqrt, scale=1.0 / D, bias=EPS),
                 reads=['ssqo%d' % tt for tt in range(g4 * 4, g4 * 4 + 4)], writes=['rso%d' % g4])
            P.op('dve', lambda e, g4=g4: e.reciprocal(rso[:, g4 * 4:(g4 + 1) * 4], rso[:, g4 * 4:(g4 + 1) * 4]), reads=['rso%d' % g4], writes=['rso%d' % g4])
            pend.extend(range(g4 * 4, g4 * 4 + 4))
    while pend:
        fin_tile(pend.pop(0))
    P.emit()
    return nc, dump_d


def _bf(a):
    return np.ascontiguousarray(np.asarray(a, dtype=np.float32)).astype(ml_dtypes.bfloat16)


def host_consts():
    c = {}
    eye = np.eye(128, dtype=np.float32)
    c["identb"] = _bf(eye)
    c["identf"] = eye
    up = (np.arange(128)[:, None] <= np.arange(128)[None, :]).astype(np.float32)
    c["U"] = up
    c["tri01"] = _bf(up)
    c["cmask"] = _bf(np.where(np.arange(128)[:, None] > np.arange(128)[None, :], -30000.0, 0.0))
    pos = np.arange(S)
    kaug = np.zeros((11, 8, S), np.float32)
    qaug = np.zeros((3, 8, S), np.float32)
    for h in range(8):
        slope = 2.0 ** (-(h + 1))
        for r in range(8):
            kaug[r, h] = np.where(pos // 256 == r, 32768.0, 0.0)
        kaug[8, h] = 1.0
        kaug[9, h] = slope * 128.0 * (pos // 128)
        kaug[10, h] = slope * (pos % 128)
        qaug[0, h] = -slope * pos
        qaug[1, h] = 1.0
        qaug[2, h] = 1.0
    c["kaugc"] = _bf(kaug)
    c["qaugc"] = _bf(qaug)
    sel = np.zeros((8, 8, 64), np.float32)
    for h in range(8):
        sel[h, h, :] = 1.0
    c["selc"] = _bf(sel)
    return c


def host_inputs(inp, b, consts=None):
    f = lambda a: np.ascontiguousarray(np.asarray(a, dtype=np.float32))
    m = dict(consts if consts is not None else host_consts())
    m["x"] = f(inp["x"][b])
    m["cT"] = f(np.asarray(inp["c"][b]).reshape(8, 128).T)
    m["w_ada"] = f(inp["w_ada"][0])
    bada = np.asarray(inp["b_ada"][0])
    m["bss"] = f(bada[:2048].reshape(16, 128).T)
    m["bgate"] = f(np.broadcast_to(bada[2048:][None, :], (128, D)))
    m["gnT"] = f(np.asarray(inp["g_norm"][0]).reshape(8, 128).T)
    m["w_in"] = f(inp["w_in"][0])
    m["convw"] = f(np.asarray(inp["conv_w"][0]).reshape(4, 8, 128).transpose(2, 1, 0).reshape(128, 32))
    m["convb"] = f(np.asarray(inp["conv_b"][0]).reshape(8, 128).T)
    bi = np.asarray(inp["b_igate"][0]); bfv = np.asarray(inp["b_fgate"][0])
    m["bif"] = f(np.broadcast_to(np.tile(np.concatenate([bi, bfv]), NT)[None, :], (128, 128)))
    m["gml"] = f(np.asarray(inp["g_mlstm_head"][0]).reshape(4, 128).T)
    m["w_out"] = f(inp["w_out"][0])
    m["gfin"] = f(np.broadcast_to(np.asarray(inp["g_final"])[None, :], (128, D)))
    return m


_CACHE = {}


def kernel(**inputs):
    consts = host_consts()
    in_maps = [host_inputs(inputs, b, consts) for b in range(8)]
    nc, _ = build_program()
    res = run_bass_kernel_spmd(nc, in_maps, core_ids=list(range(8)))
    out = np.stack([np.asarray(r["out"], dtype=np.float32) for r in res.results], axis=0)
    return out
```
